# Optimizing a Trainium2 kernel written in Bass

```python
import jax, jax.numpy as jnp
from jax import lax
import numpy as np

D_MODEL = 1024
BATCH = 8
SEQ = 4096
DEPTH = 2

MEM_LEN = 256
EPS = 1e-6
CONV_WIDTH = 4
SSD_D_INNER = D_MODEL
SSD_HEAD_DIM = 64
SSD_HEADS = SSD_D_INNER // SSD_HEAD_DIM
SSD_GROUPS = 2
SSD_STATE = 128
SSD_XBC = SSD_D_INNER + 2 * SSD_GROUPS * SSD_STATE
SSD_CHUNK = 128
MLSTM_D_INNER = D_MODEL
MLSTM_HEADS = 4
MLSTM_HEAD_DIM = MLSTM_D_INNER // MLSTM_HEADS
MLSTM_CHUNK = 128
EVEN_IN = SSD_D_INNER + SSD_XBC + SSD_HEADS + 2 * MLSTM_D_INNER + 2 * MLSTM_HEADS
HGRN_D_INNER = D_MODEL
HGRN_HEAD_DIM = 128
HGRN_HEADS = HGRN_D_INNER // HGRN_HEAD_DIM
HGRN_CHUNK = 64
XATTN_HEADS = 4
XATTN_HEAD_DIM = D_MODEL // XATTN_HEADS
PEER_HEADS = 8
PEER_N_KEYS = 128
PEER_N_EXPERTS = PEER_N_KEYS * PEER_N_KEYS
PEER_TOPK = 16
PEER_QUERY_DIM = 256
PEER_HALF = PEER_QUERY_DIM // 2
PEER_BLOCK = 128
N_EVEN = (DEPTH + 1) // 2
N_ODD = DEPTH // 2

kernel_name = 'hybrid_ssd_mlstm_hgrn2_peer_trunk'

F32 = jnp.float32


def head_rms_norm(x, w, n_groups):
    shp = x.shape
    x32 = x.astype(F32).reshape(shp[:-1] + (n_groups, shp[-1] // n_groups))
    y = x32 * lax.rsqrt(jnp.mean(x32 * x32, axis=-1, keepdims=True) + EPS)
    return (y.reshape(shp) * w.astype(F32)).astype(x.dtype)


def rms_norm(x, w):
    return head_rms_norm(x, w, 1)


def causal_conv(x, w, b):
    y = lax.conv_general_dilated(x, w[:, None, :], window_strides=(1,), padding=[(w.shape[0] - 1, 0)],
                                 dimension_numbers=('NWC', 'WIO', 'NWC'), feature_group_count=x.shape[-1])
    return y + b


def ssd_mixer(z, xbc, dt_raw, conv_w, conv_b, dt_bias, a_log, d_skip, norm_w):
    bsz, seq, _ = z.shape
    L = SSD_CHUNK; nc = seq // L; G = SSD_GROUPS; E = SSD_HEADS // SSD_GROUPS
    P = SSD_HEAD_DIM; N = SSD_STATE
    xbc = jax.nn.silu(causal_conv(xbc, conv_w, conv_b))
    xs = xbc[..., :SSD_D_INNER].astype(F32).reshape(bsz, nc, L, G, E, P)
    bm = xbc[..., SSD_D_INNER:SSD_D_INNER + G * N].astype(F32).reshape(bsz, nc, L, G, N)
    cm = xbc[..., SSD_D_INNER + G * N:].astype(F32).reshape(bsz, nc, L, G, N)
    dt = jax.nn.softplus(dt_raw.astype(F32) + dt_bias.astype(F32)).reshape(bsz, nc, L, G, E)
    a = -jnp.exp(a_log.astype(F32)).reshape(G, E)
    cs = jnp.cumsum(jnp.transpose(dt * a, (0, 3, 4, 1, 2)), axis=-1)
    x_dt = xs * dt[..., None]
    causal = jnp.tril(jnp.ones((L, L), dtype=bool))
    decay = jnp.exp(jnp.where(causal, cs[..., :, None] - cs[..., None, :], -jnp.inf))
    cb = jnp.einsum('bclgn,bcsgn->bgcls', cm, bm)
    y_diag = jnp.einsum('bgcls,bgecls,bcsgep->bclgep', cb, decay, x_dt)
    decay_to_end = jnp.exp(cs[..., -1:] - cs)
    chunk_states = jnp.einsum('bcsgn,bgecs,bcsgep->bcgepn', bm, decay_to_end, x_dt)
    chunk_decay = jnp.exp(cs[..., -1])

    def step(state, inp):
        s_c, a_c = inp
        return a_c[..., None, None] * state + s_c, state

    init = jnp.zeros((bsz, G, E, P, N), F32)
    _, prev = lax.scan(step, init, (jnp.moveaxis(chunk_states, 1, 0), jnp.moveaxis(chunk_decay, 3, 0)))
    y_off = jnp.einsum('bclgn,cbgepn,bgecl->bclgep', cm, prev, jnp.exp(cs))
    y = y_diag + y_off + xs * d_skip.astype(F32).reshape(G, E, 1)
    y = y.reshape(bsz, seq, SSD_D_INNER) * jax.nn.silu(z.astype(F32))
    return head_rms_norm(y, norm_w, G).astype(z.dtype)


def mlstm_mixer(xm, og, ig, fg, conv_w, conv_b, wq, wk, wv, i_bias, f_bias, norm_w):
    bsz, seq, _ = xm.shape
    H = MLSTM_HEADS; Dh = MLSTM_HEAD_DIM; L = MLSTM_CHUNK; nc = seq // L
    xc = jax.nn.silu(causal_conv(xm, conv_w, conv_b))

    def heads(t, w):
        y = jnp.einsum('bshd,hde->bhse', t.reshape(bsz, seq, H, Dh), w)
        return y.astype(F32).reshape(bsz, H, nc, L, Dh)

    q = heads(xc, wq)
    k = heads(xc, wk) * (Dh ** -0.5)
    v = heads(xm, wv)
    i_t = jnp.transpose(ig.astype(F32) + i_bias.astype(F32), (0, 2, 1)).reshape(bsz, H, nc, L)
    logf = jnp.transpose(jax.nn.log_sigmoid(fg.astype(F32) + f_bias.astype(F32)), (0, 2, 1)).reshape(bsz, H, nc, L)
    bcum = jnp.cumsum(logf, axis=-1)
    causal = jnp.tril(jnp.ones((L, L), dtype=bool))
    dlog = jnp.where(causal, bcum[..., :, None] - bcum[..., None, :] + i_t[..., None, :], -jnp.inf)
    m_intra = jnp.max(dlog, axis=-1)
    g_end = bcum[..., -1:] - bcum + i_t
    m_chunk = jnp.max(g_end, axis=-1)
    w_end = jnp.exp(g_end - m_chunk[..., None])
    c_local = jnp.einsum('bhcs,bhcsd,bhcse->bhcde', w_end, k, v)
    n_local = jnp.einsum('bhcs,bhcsd->bhcd', w_end, k)
    b_end = bcum[..., -1]

    def step(carry, inp):
        c_st, n_st, m_st = carry
        be, mc, cl, nl = inp
        m_new = jnp.maximum(be + m_st, mc)
        a_old = jnp.exp(be + m_st - m_new)
        a_new = jnp.exp(mc - m_new)
        c_new = a_old[..., None, None] * c_st + a_new[..., None, None] * cl
        n_new = a_old[..., None] * n_st + a_new[..., None] * nl
        return (c_new, n_new, m_new), (c_st, n_st, m_st)

    init = (jnp.zeros((bsz, H, Dh, Dh), F32), jnp.zeros((bsz, H, Dh), F32), jnp.zeros((bsz, H), F32))
    mv = lambda t: jnp.moveaxis(t, 2, 0)
    _, (c_prev, n_prev, m_prev) = lax.scan(step, init, (mv(b_end), mv(m_chunk), mv(c_local), mv(n_local)))
    m_prev_b = jnp.moveaxis(m_prev, 0, 2)[..., None]
    m_t = jnp.maximum(bcum + m_prev_b, m_intra)
    a_t = jnp.exp(bcum + m_prev_b - m_t)
    qk = jnp.einsum('bhcld,bhcsd->bhcls', q, k) * jnp.exp(dlog - m_t[..., None])
    num = a_t[..., None] * jnp.einsum('bhcld,cbhde->bhcle', q, c_prev) + jnp.einsum('bhcls,bhcse->bhcle', qk, v)
    den = a_t * jnp.einsum('bhcld,cbhd->bhcl', q, n_prev) + jnp.sum(qk, axis=-1)
    hid = num / jnp.maximum(jnp.abs(den), jnp.exp(-m_t))[..., None]
    hid = jnp.transpose(hid, (0, 2, 3, 1, 4)).reshape(bsz, seq, H * Dh)
    hid = head_rms_norm(hid, norm_w, H) * jax.nn.sigmoid(og.astype(F32))
    return hid.astype(xm.dtype)


def hgrn2_mixer(uq, uf, ui, ug, lb, norm_w):
    bsz, seq, _ = uq.shape
    H = HGRN_HEADS; Dk = HGRN_HEAD_DIM; L = HGRN_CHUNK; nc = seq // L

    def heads(t):
        return jnp.transpose(t.reshape(bsz, nc, L, H, -1), (0, 3, 1, 2, 4))

    uf32 = uf.astype(F32)
    lb = lb.astype(F32)
    logf = heads(jnp.log(lb + (1.0 - lb) * jax.nn.sigmoid(uf32)))
    k = heads((1.0 - lb) * jax.nn.sigmoid(-uf32))
    q = heads(jax.nn.silu(uq.astype(F32)))
    v = heads(ui.astype(F32))
    gc = jnp.cumsum(logf, axis=3)
    g_ref = gc[:, :, :, L // 2:L // 2 + 1, :]
    causal = jnp.tril(jnp.ones((L, L), dtype=bool))
    att = jnp.einsum('bhcld,bhcsd->bhcls', q * jnp.exp(gc - g_ref), k * jnp.exp(g_ref - gc))
    att = jnp.where(causal, att, 0.0)
    o = jnp.einsum('bhcls,bhcse->bhcle', att, v)
    s_local = jnp.einsum('bhcsd,bhcse->bhcde', k * jnp.exp(gc[..., -1:, :] - gc), v)
    chunk_decay = jnp.exp(gc[..., -1, :])

    def step(state, inp):
        a_c, s_c = inp
        return a_c[..., :, None] * state + s_c, state

    init = jnp.zeros((bsz, H, Dk, Dk), F32)
    _, s_prev = lax.scan(step, init, (jnp.moveaxis(chunk_decay, 2, 0), jnp.moveaxis(s_local, 2, 0)))
    o = o + jnp.einsum('bhcld,cbhde->bhcle', q * jnp.exp(gc), s_prev)
    o = jnp.transpose(o, (0, 2, 3, 1, 4)).reshape(bsz, seq, HGRN_D_INNER)
    o = head_rms_norm(o, norm_w, H) * jax.nn.silu(ug.astype(F32))
    return o.astype(uq.dtype)


def even_mixer(hn, w_in, w_out, ssd_conv_w, ssd_conv_b, ssd_dt_bias, ssd_a_log, ssd_d_skip, ssd_norm,
               ml_conv_w, ml_conv_b, ml_wq, ml_wk, ml_wv, ml_i_bias, ml_f_bias, ml_norm):
    proj = hn @ w_in
    o1 = SSD_D_INNER
    o2 = o1 + SSD_XBC
    o3 = o2 + SSD_HEADS
    o4 = o3 + MLSTM_D_INNER
    o5 = o4 + MLSTM_D_INNER
    o6 = o5 + MLSTM_HEADS
    z, xbc, dt_raw, xm, og, ig, fg = jnp.split(proj, [o1, o2, o3, o4, o5, o6], axis=-1)
    y_a = ssd_mixer(z, xbc, dt_raw, ssd_conv_w, ssd_conv_b, ssd_dt_bias, ssd_a_log, ssd_d_skip, ssd_norm)
    y_b = mlstm_mixer(xm, og, ig, fg, ml_conv_w, ml_conv_b, ml_wq, ml_wk, ml_wv, ml_i_bias, ml_f_bias, ml_norm)
    return jnp.concatenate([y_a, y_b], axis=-1).astype(hn.dtype) @ w_out


def odd_mixer(hn, w_in, w_out, lb, norm_w):
    uq, uf, ui, ug = jnp.split(hn @ w_in, 4, axis=-1)
    return hgrn2_mixer(uq, uf, ui, ug, lb, norm_w).astype(hn.dtype) @ w_out


def mem_cross_attention(hn, mem_n, wq, wkv, wo):
    bsz, seq, _ = hn.shape
    q = (hn @ wq).reshape(bsz, seq, XATTN_HEADS, XATTN_HEAD_DIM)
    k, v = jnp.split(mem_n @ wkv, 2, axis=-1)
    k = k.reshape(bsz, -1, XATTN_HEADS, XATTN_HEAD_DIM)
    v = v.reshape(bsz, -1, XATTN_HEADS, XATTN_HEAD_DIM)
    scores = jnp.einsum('bshd,bmhd->bhsm', q, k).astype(F32) * (XATTN_HEAD_DIM ** -0.5)
    p = jax.nn.softmax(scores, axis=-1).astype(v.dtype)
    o = jnp.einsum('bhsm,bmhd->bshd', p, v).reshape(bsz, seq, D_MODEL)
    return o @ wo


def peer_ffn(hn, wq, subkeys, u_tab, v_tab):
    bsz, seq, _ = hn.shape
    T = bsz * seq
    xt = hn.reshape(T, D_MODEL)
    q = (xt @ wq).reshape(T, PEER_HEADS, 2, PEER_HALF)
    s = jnp.einsum('thpd,hpkd->thpk', q, subkeys).astype(F32)
    s1, i1 = lax.top_k(s[:, :, 0], PEER_TOPK)
    s2, i2 = lax.top_k(s[:, :, 1], PEER_TOPK)
    cand = (s1[..., :, None] + s2[..., None, :]).reshape(T, PEER_HEADS, PEER_TOPK * PEER_TOPK)
    cand_idx = (i1[..., :, None] * PEER_N_KEYS + i2[..., None, :]).reshape(T, PEER_HEADS, PEER_TOPK * PEER_TOPK)
    top_s, pos = lax.top_k(cand, PEER_TOPK)
    idx = jnp.take_along_axis(cand_idx, pos, axis=-1)
    gate = jax.nn.softmax(top_s, axis=-1)
    nb = T // PEER_BLOCK

    def expert_block(args):
        xb, ib, gb = args
        ub = u_tab[ib]
        vb = v_tab[ib]
        act = jax.nn.gelu(jnp.einsum('td,thkd->thk', xb, ub).astype(F32), approximate=False)
        return jnp.einsum('thk,thkd->td', (gb * act).astype(vb.dtype), vb)

    out = lax.map(expert_block, (xt.reshape(nb, PEER_BLOCK, D_MODEL),
                                 idx.reshape(nb, PEER_BLOCK, PEER_HEADS, PEER_TOPK),
                                 gate.reshape(nb, PEER_BLOCK, PEER_HEADS, PEER_TOPK)))
    return out.reshape(bsz, seq, D_MODEL).astype(hn.dtype)


def setup_inputs(seed: int = 0) -> dict:
    key = jax.random.key(seed)
    ks = jax.random.split(key, 40)
    nrm = lambda k, shape, scale: jax.random.normal(k, shape, F32) * scale
    gain = lambda k, shape: 1.0 + 0.02 * jax.random.normal(k, shape, F32)
    dt = jnp.exp(jax.random.uniform(ks[10], (N_EVEN, SSD_HEADS), F32) * (np.log(0.1) - np.log(0.001)) + np.log(0.001))
    f_bias = jnp.linspace(3.0, 6.0, MLSTM_HEADS, dtype=F32)[None, :] + 0.1 * jax.random.normal(ks[21], (N_EVEN, MLSTM_HEADS), F32)
    return {
        'x': nrm(ks[0], (BATCH, SEQ, D_MODEL), 1.0),
        'mem': nrm(ks[1], (BATCH, MEM_LEN, D_MODEL), 1.0),
        'norm_mix': gain(ks[2], (DEPTH, D_MODEL)),
        'norm_xattn': gain(ks[3], (DEPTH, D_MODEL)),
        'norm_mem': gain(ks[4], (DEPTH, D_MODEL)),
        'norm_ffn': gain(ks[5], (DEPTH, D_MODEL)),
        'norm_final': gain(ks[6], (D_MODEL,)),
        'ev_w_in': nrm(ks[7], (N_EVEN, D_MODEL, EVEN_IN), D_MODEL ** -0.5),
        'ev_w_out': nrm(ks[8], (N_EVEN, SSD_D_INNER + MLSTM_D_INNER, D_MODEL), (SSD_D_INNER + MLSTM_D_INNER) ** -0.5),
        'ssd_conv_w': nrm(ks[9], (N_EVEN, CONV_WIDTH, SSD_XBC), CONV_WIDTH ** -0.5),
        'ssd_conv_b': nrm(ks[11], (N_EVEN, SSD_XBC), 0.02),
        'ssd_dt_bias': dt + jnp.log(-jnp.expm1(-dt)),
        'ssd_a_log': jnp.log(jax.random.uniform(ks[12], (N_EVEN, SSD_HEADS), F32, 1.0, 16.0)),
        'ssd_d_skip': 1.0 + 0.1 * jax.random.normal(ks[13], (N_EVEN, SSD_HEADS), F32),
        'ssd_norm': gain(ks[14], (N_EVEN, SSD_D_INNER)),
        'ml_conv_w': nrm(ks[15], (N_EVEN, CONV_WIDTH, MLSTM_D_INNER), CONV_WIDTH ** -0.5),
        'ml_conv_b': nrm(ks[16], (N_EVEN, MLSTM_D_INNER), 0.02),
        'ml_wq': nrm(ks[17], (N_EVEN, MLSTM_HEADS, MLSTM_HEAD_DIM, MLSTM_HEAD_DIM), MLSTM_HEAD_DIM ** -0.5),
        'ml_wk': nrm(ks[18], (N_EVEN, MLSTM_HEADS, MLSTM_HEAD_DIM, MLSTM_HEAD_DIM), MLSTM_HEAD_DIM ** -0.5),
        'ml_wv': nrm(ks[19], (N_EVEN, MLSTM_HEADS, MLSTM_HEAD_DIM, MLSTM_HEAD_DIM), MLSTM_HEAD_DIM ** -0.5),
        'ml_i_bias': nrm(ks[20], (N_EVEN, MLSTM_HEADS), 0.1),
        'ml_f_bias': f_bias,
        'ml_norm': gain(ks[22], (N_EVEN, MLSTM_D_INNER)),
        'od_w_in': nrm(ks[23], (N_ODD, D_MODEL, 4 * HGRN_D_INNER), D_MODEL ** -0.5),
        'od_w_out': nrm(ks[24], (N_ODD, HGRN_D_INNER, D_MODEL), HGRN_D_INNER ** -0.5),
        'hgrn_lb_logits': nrm(ks[25], (DEPTH, HGRN_D_INNER), 0.1),
        'hgrn_norm': gain(ks[26], (N_ODD, HGRN_D_INNER)),
        'xa_wq': nrm(ks[27], (DEPTH, D_MODEL, D_MODEL), D_MODEL ** -0.5),
        'xa_wkv': nrm(ks[28], (DEPTH, D_MODEL, 2 * D_MODEL), D_MODEL ** -0.5),
        'xa_wo': nrm(ks[29], (DEPTH, D_MODEL, D_MODEL), D_MODEL ** -0.5),
        'peer_wq': nrm(ks[30], (DEPTH, D_MODEL, PEER_HEADS * PEER_QUERY_DIM), D_MODEL ** -0.5),
        'peer_subkeys': nrm(ks[31], (DEPTH, PEER_HEADS, 2, PEER_N_KEYS, PEER_HALF), PEER_HALF ** -0.5),
        'peer_u': nrm(ks[32], (DEPTH, PEER_N_EXPERTS, D_MODEL), D_MODEL ** -0.5),
        'peer_v': nrm(ks[33], (DEPTH, PEER_N_EXPERTS, D_MODEL), (PEER_HEADS * PEER_TOPK) ** -0.5),
    }


def reference(x, mem, norm_mix, norm_xattn, norm_mem, norm_ffn, norm_final,
              ev_w_in, ev_w_out, ssd_conv_w, ssd_conv_b, ssd_dt_bias, ssd_a_log, ssd_d_skip, ssd_norm,
              ml_conv_w, ml_conv_b, ml_wq, ml_wk, ml_wv, ml_i_bias, ml_f_bias, ml_norm,
              od_w_in, od_w_out, hgrn_lb_logits, hgrn_norm,
              xa_wq, xa_wkv, xa_wo,
              peer_wq, peer_subkeys, peer_u, peer_v):
    lb_cum = jnp.cumsum(jax.nn.softmax(hgrn_lb_logits.astype(F32), axis=0), axis=0)
    h = x
    for layer in range(DEPTH):
        hn = rms_norm(h, norm_mix[layer])
        if layer % 2 == 0:
            e = layer // 2
            mix = even_mixer(hn, ev_w_in[e], ev_w_out[e], ssd_conv_w[e], ssd_conv_b[e], ssd_dt_bias[e],
                             ssd_a_log[e], ssd_d_skip[e], ssd_norm[e], ml_conv_w[e], ml_conv_b[e],
                             ml_wq[e], ml_wk[e], ml_wv[e], ml_i_bias[e], ml_f_bias[e], ml_norm[e])
        else:
            o = layer // 2
            mix = odd_mixer(hn, od_w_in[o], od_w_out[o], lb_cum[layer - 1], hgrn_norm[o])
        h = h + mix.astype(h.dtype)
        mem_n = rms_norm(mem, norm_mem[layer])
        h = h + mem_cross_attention(rms_norm(h, norm_xattn[layer]), mem_n, xa_wq[layer], xa_wkv[layer], xa_wo[layer]).astype(h.dtype)
        h = h + peer_ffn(rms_norm(h, norm_ffn[layer]), peer_wq[layer], peer_subkeys[layer], peer_u[layer], peer_v[layer]).astype(h.dtype)
    return rms_norm(h, norm_final)
```

```python
import numpy as np
from contextlib import ExitStack
import concourse.bass as bass
import concourse.mybir as mybir
from concourse.bass_utils import run_bass_kernel_spmd

F32 = mybir.dt.float32
BF16 = mybir.dt.bfloat16
U32 = mybir.dt.uint32
I32 = mybir.dt.int32
AF = mybir.ActivationFunctionType
ALU = mybir.AluOpType
AX = mybir.AxisListType

NDS = 24
WRITE_KEYS = ('out', 'accum_out', 'ap', 'out_max', 'out_indices')
EPS = 1e-6


class KB:
    def __init__(self):
        self.nc = bass.Bass("TRN2", target_bir_lowering=False)
        nc = self.nc
        self.engs = {'pe': nc.tensor, 'dve': nc.vector, 'act': nc.scalar, 'pool': nc.gpsimd, 'sp': nc.sync}
        self.sem = {k: nc.alloc_semaphore("s_" + k) for k in self.engs}
        self.cnt = {k: 0 for k in self.engs}
        self.seen = {k: {} for k in self.engs}
        self.dma_sems = [nc.alloc_semaphore("d%d" % i) for i in range(NDS)]
        self.dma_val = [0] * NDS
        self.dma_rr = 0
        self.bufs = {}
        self.gran = {}
        self.psum_names = set()
        self.rec = None
        self.self_skip = {}
        self.n_ins = 0
        self._uid = 0
        self.stack = None
        self._scope = None
        self.profile_scopes = False

    def sb(self, shape, dt=F32, name=None, gran=None):
        self._uid += 1
        nm = "%s_%d" % (name or "sb", self._uid)
        if gran is not None:
            self.gran[nm] = gran * (2 if dt == BF16 else 4)
        if self.stack is not None:
            return self.stack.enter_context(self.nc.sbuf_tensor(nm, list(shape), dt))
        return self.nc.alloc_sbuf_tensor(nm, list(shape), dt)

    def ps(self, shape, dt=F32, name=None, gran=None):
        self._uid += 1
        nm = "%s_%d" % (name or "ps", self._uid)
        self.gran[nm] = 2048
        self.psum_names.add(nm)
        if self.stack is not None:
            return self.stack.enter_context(self.nc.psum_tensor(nm, list(shape), dt))
        return self.nc.alloc_psum_tensor(nm, list(shape), dt)

    def ring(self, n, shape, dt=F32, name=None, psum=False, gran=None):
        return [(self.ps if psum else self.sb)(shape, dt, name, gran=gran) for _ in range(n)]

    def begin_stage(self):
        self.barrier()
        self.stack = ExitStack()

    def end_stage(self):
        self.barrier()
        self.stack.close()
        self.stack = None

    def scope(self, name):
        if getattr(self, '_scope', None) is not None:
            self.nc.leave_named_scope(self._scope_tok) if False else self._scope.__exit__(None, None, None)
            self._scope = None
        if name is not None and self.profile_scopes:
            self._scope = self.nc.named_scope(name)
            self._scope.__enter__()

    def dram(self, name, shape, dt=F32, kind="Internal"):
        return self.nc.dram_tensor(name, list(shape), dt, kind=kind)

    def semh(self, sk):
        return self.sem[sk[1]] if sk[0] == 'e' else self.dma_sems[sk[1]]

    def slots(self, ap):
        t = ap.tensor
        n = t.name
        g = self.gran.get(n)
        if g is None:
            return [n]
        apl = list(ap.ap)
        F_ = int(apl[0][0])
        isz = 2 if ap.dtype == BF16 else 4
        off = int(ap.offset) % F_ if F_ > 0 else int(ap.offset)
        lo = hi = off
        for (st, num) in apl[1:]:
            st = int(st); num = int(num)
            if st >= 0:
                hi += st * (num - 1)
            else:
                lo += st * (num - 1)
        return [(n, j) for j in range(lo * isz // g, hi * isz // g + 1)]

    def bufl(self, ap):
        out = []
        for key in self.slots(ap):
            b = self.bufs.get(key)
            if b is None:
                b = {'w': None, 'r': {}}
                self.bufs[key] = b
            out.append(b)
        return out

    def emit(self, eng, fn, reads, writes, is_dma=False):
        deps = []
        rb = [b for t in reads for b in self.bufl(t)]
        wb = [b for t in writes for b in self.bufl(t)]
        for t in reads:
            if t.tensor.name in self.psum_names:
                for b in self.bufl(t):
                    deps.extend((sk_, v_) for sk_, v_ in b['r'].items() if sk_ != ('e', eng))
        for b in rb:
            if b['w']:
                deps.append(b['w'])
        for b in wb:
            if b['w']:
                deps.append(b['w'])
            deps.extend(b['r'].items())
        E = self.engs[eng]
        seen = self.seen[eng]
        for (sk, val) in deps:
            if eng == 'pe' and sk == ('e', 'pe'):
                continue
            if seen.get(sk, 0) >= val:
                continue
            if sk == ('e', eng) and eng in self.self_skip and self.cnt[eng] - val >= self.self_skip[eng]:
                continue
            E.wait_ge(self.semh(sk), val)
            seen[sk] = val
        if is_dma:
            i = self.dma_rr
            self.dma_rr = (i + 1) % NDS
            sk = ('d', i)
            pv = self.dma_val[i]
            if pv and seen.get(sk, 0) < pv:
                E.wait_ge(self.dma_sems[i], pv)
                seen[sk] = pv
            ins = fn(E)
            ins.then_inc(self.dma_sems[i], 16)
            self.dma_val[i] += 16
            tok = (sk, self.dma_val[i])
        else:
            ins = fn(E)
            ins.then_inc(self.sem[eng], 1)
            self.cnt[eng] += 1
            tok = (('e', eng), self.cnt[eng])
        self.n_ins += 1
        for b in rb:
            r = b['r']
            if r.get(tok[0], 0) < tok[1]:
                r[tok[0]] = tok[1]
        for b in wb:
            b['w'] = tok
            b['r'] = {}
        return ins

    def op(self, eng, name, **kw):
        if self.rec is not None:
            c = 0
            if eng == 'dve':
                o = kw.get('out', kw.get('ap'))
                n = 1
                if o is not None:
                    for d_ in list(o.shape)[1:]:
                        n *= int(d_)
                c = 70 + (2 * n if name == 'tensor_tensor' else n)
            self.rec.append((eng, name, kw, c))
            return None
        reads = [v for k_, v in kw.items() if k_ not in WRITE_KEYS and hasattr(v, 'tensor')]
        writes = [v for k_, v in kw.items() if k_ in WRITE_KEYS and v is not None and hasattr(v, 'tensor')]
        return self.emit(eng, lambda E: getattr(E, name)(**kw), reads, writes, is_dma=(name == 'dma_start'))

    def replay(self, ops, n=None):
        assert self.rec is None
        if n is None:
            n = len(ops)
        for (eng, name, kw, c) in ops[:n]:
            if eng != 'marker':
                self.op(eng, name, **kw)
        return ops[n:]

    def replay_cost(self, ops, budget, chunk=None):
        acc = 0
        j = 0
        while j < len(ops) and acc < budget:
            eng, name, kw, c = ops[j]
            if eng == 'marker':
                if chunk is not None and chunk < name:
                    break
                j += 1
                continue
            self.op(eng, name, **kw)
            acc += c
            j += 1
        return ops[j:]

    def dma(self, out, in_, eng='sp', **kw):
        return self.op(eng, 'dma_start', out=out, in_=in_, **kw)

    def barrier(self):
        for eng, E in self.engs.items():
            seen = self.seen[eng]
            for e2 in self.engs:
                sk = ('e', e2)
                v = self.cnt[e2]
                if e2 == eng and eng == 'pe':
                    continue
                if v and seen.get(sk, 0) < v:
                    E.wait_ge(self.sem[e2], v)
                    seen[sk] = v
            for i in range(NDS):
                sk = ('d', i)
                v = self.dma_val[i]
                if v and seen.get(sk, 0) < v:
                    E.wait_ge(self.dma_sems[i], v)
                    seen[sk] = v
        for b in self.bufs.values():
            b['w'] = None
            b['r'] = {}

    def mm(self, out, lhsT, rhs, start=True, stop=True):
        return self.op('pe', 'matmul', out=out, lhsT=lhsT, rhs=rhs, start=start, stop=stop)

    def tr(self, out, in_, ident):
        return self.op('pe', 'transpose', out=out, in_=in_, identity=ident)

    def act(self, out, in_, func, bias=None, scale=None, accum_out=None, eng='act'):
        kw = dict(out=out, in_=in_, func=func)
        if bias is not None:
            kw['bias'] = bias
        if scale is not None:
            kw['scale'] = scale
        if accum_out is not None:
            kw['accum_out'] = accum_out
        return self.op('act', 'activation', **kw)

    def tt(self, out, in0, in1, op, eng='dve'):
        return self.op(eng, 'tensor_tensor', out=out, in0=in0, in1=in1, op=op)

    def ts(self, out, in0, s1, op0, s2=None, op1=None, eng='dve', accum_out=None):
        kw = dict(out=out, in0=in0, scalar1=s1, scalar2=s2, op0=op0)
        if op1 is not None:
            kw['op1'] = op1
        if accum_out is not None:
            kw['accum_out'] = accum_out
        return self.op(eng, 'tensor_scalar', **kw)

    def stt(self, out, in0, scalar, in1, op0, op1):
        return self.op('dve', 'scalar_tensor_tensor', out=out, in0=in0, scalar=scalar, in1=in1, op0=op0, op1=op1)

    def copy(self, out, in_, eng='dve'):
        if eng == 'act':
            return self.op('act', 'activation', out=out, in_=in_, func=AF.Copy)
        return self.op(eng, 'tensor_copy', out=out, in_=in_)

    def memset(self, ap, c, eng='dve'):
        return self.op(eng, 'memset', ap=ap, constant=c)


D = 1024
E_IN = 4632


class MK(KB):
    def __init__(self, T, dbg=()):
        super().__init__()
        self.T = T
        self.NT = T // 128
        self.dbg = set(dbg)
        self.inputs = {}
        self.scr = {}
        self.x_pool_every = 10 ** 9; self.x_pm_eng = 'dve'; self.x_v_eng = 'act'; self.x_st_eng = 'pool'

    def inp(self, name, shape, dt=F32):
        t = self.dram(name, shape, dt, kind="ExternalInput")
        self.inputs[name] = t
        return t

    def scratch(self, name, shape, dt=F32):
        t = self.dram(name, shape, dt, kind=("ExternalOutput" if name in self.dbg else "Internal"))
        self.scr[name] = t
        return t

    def make_consts(self):
        k = self
        self.ones = k.sb([128, 128], F32, "ones")
        k.memset(self.ones[:], 1.0)
        self.idf = k.sb([128, 128], F32, "idf")
        k.op('pool', 'affine_select', out=self.idf[:], in_=self.ones[:], pattern=[[-1, 128]],
             compare_op=ALU.is_equal, fill=0.0, base=0, channel_multiplier=1)
        self.ident = k.sb([128, 128], BF16, "ident")
        k.copy(self.ident[:], self.idf[:])
        self.triu = k.sb([128, 128], F32, "triu")
        k.op('pool', 'affine_select', out=self.triu[:], in_=self.ones[:], pattern=[[1, 128]],
             compare_op=ALU.is_ge, fill=0.0, base=0, channel_multiplier=-1)
        self.ntriu = k.sb([128, 128], F32, "ntriu")
        k.ts(self.ntriu[:], self.triu[:], -1.0, ALU.mult)
        self.mhalf = k.sb([128, 16], F32, "mhalf")
        k.memset(self.mhalf[:], -0.5)
        self.zeros = k.sb([128, 512], F32, "zeros")
        k.memset(self.zeros[:], 0.0)

    def bcast_row(self, dst, src_ap_1d):
        self.dma(dst, src_ap_1d.partition_broadcast(128))

    def rms_T(self, xt, gb, hn, hT, tp, ss, sq):
        k = self
        k.act(sq[:], xt[:], AF.Square, accum_out=ss[:, 0:1])
        k.ts(ss[:, 1:2], ss[:, 0:1], 1.0 / D, ALU.mult, EPS, ALU.add)
        k.tt(ss[:, 3:4], ss[:, 1:2], self.mhalf[:, 0:1], ALU.pow, eng='pool')
        k.stt(hn[:], xt[:], ss[:, 3:4], gb[:], ALU.mult, ALU.mult)
        for c in range(8):
            k.tr(tp[:, c, :], hn[:, c * 128:(c + 1) * 128], self.ident[:])
        k.copy(hT, tp[:], eng='act')

    def stage_inproj_even(self, h_src, w_in, gamma, side_fn=None):
        k = self
        T, NT = self.T, self.NT
        z_s = k.scratch("z_s", [T, 1024])
        cT_s = k.scratch("cT_s", [2560, T + 3])
        dt_s = k.scratch("dt_s", [T, 16])
        og_s = k.scratch("og_s", [T, 1024])
        gT_s = k.scratch("gT_s", [8, T])
        k.begin_stage()
        W = k.sb([128, 8, E_IN], BF16, "Win")
        for c in range(8):
            k.dma(W[:, c, :], w_in[c * 128:(c + 1) * 128, :], eng='pool')
        gb = k.sb([128, D], F32, "gb")
        k.bcast_row(gb[:], gamma)
        for c in range(20):
            k.dma(cT_s.ap()[c * 128:(c + 1) * 128, 0:3], self.zeros[:, 0:3])
        xts = k.ring(2, [128, D], F32, "xt")
        sqs = k.ring(1, [128, D], F32, "sq")
        sss = k.ring(2, [128, 4], F32, "ss")
        hns = k.ring(2, [128, D], BF16, "hn")
        hTs = k.ring(2, [128, 8, 128], BF16, "hT")
        tps = k.ring(1, [128, 8, 128], BF16, "tp", psum=True)
        pss = k.ring(4, [128, 512], F32, "pp", psum=True)
        psf = k.ring(2, [128, 4, 128], F32, "pf", psum=True)
        otok = k.ring(2, [128, 1024 + 16 + 1024], F32, "otok")
        ofm = k.ring(2, [128, 20, 128], F32, "ofm")
        og8 = k.ring(2, [8, 128], F32, "og8")
        groups = [(0, 512, 0), (512, 512, 512), (2560, 16, 1024), (3600, 512, 1040), (4112, 512, 1552)]
        nps = 0
        side = side_fn() if side_fn is not None else []
        per = (len(side) + NT - 1) // NT
        for i in range(NT):
            side = k.replay(side, per)
            xt = xts[i % 2]; hn = hns[i % 2]; hT = hTs[i % 2]; ss = sss[i % 2]
            k.dma(xt[:], h_src[i * 128:(i + 1) * 128, :])
            k.rms_T(xt, gb, hn, hT[:], tps[0], ss, sqs[0])
            ot = otok[i % 2]
            for (c0, n, d0) in groups:
                p = pss[nps % 4]; nps += 1
                for c in range(8):
                    k.mm(p[:, 0:n], hT[:, c, :], W[:, c, c0:c0 + n], start=(c == 0), stop=(c == 7))
                k.copy(ot[:, d0:d0 + n], p[:, 0:n], eng=('act' if nps % 2 else 'dve'))
            k.dma(z_s.ap()[i * 128:(i + 1) * 128, :], ot[:, 0:1024], eng='pool')
            k.dma(dt_s.ap()[i * 128:(i + 1) * 128, :], ot[:, 1024:1040], eng='pool')
            k.dma(og_s.ap()[i * 128:(i + 1) * 128, :], ot[:, 1040:2064], eng='pool')
            of = ofm[i % 2]
            for ch in range(20):
                f0 = 1024 + ch * 128 if ch < 12 else 2576 + (ch - 12) * 128
                p = psf[(ch // 4) % 2]
                for c in range(8):
                    k.mm(p[:, ch % 4, :], W[:, c, f0:f0 + 128], hT[:, c, :], start=(c == 0), stop=(c == 7))
                if ch % 4 == 3:
                    k.copy(of[:, ch - 3:ch + 1, :], p[:], eng=('act' if (ch // 4) % 2 else 'dve'))
            k.dma(cT_s.ap()[:, 3 + i * 128:3 + (i + 1) * 128].rearrange("(ch p) t -> p ch t", p=128), of[:], eng='pool')
            p = pss[nps % 4]; nps += 1
            for c in range(8):
                k.mm(p[0:8, 0:128], W[:, c, 4624:4632], hT[:, c, :], start=(c == 0), stop=(c == 7))
            k.copy(og8[i % 2][:], p[0:8, 0:128])
            k.dma(gT_s.ap()[:, i * 128:(i + 1) * 128], og8[i % 2][:], eng='pool')
        k.replay(side)
        k.end_stage()

    def stage_ssd(self, convwb, dt_bias, a_log, d_skip, ssd_norm, own=True):
        k = self
        T, NT = self.T, self.NT
        z_s, cT_s, dt_s = k.scr["z_s"], k.scr["cT_s"], k.scr["dt_s"]
        ya_s = k.scr["ya_s"] if "ya_s" in k.scr else k.scratch("ya_s", [T, 2048], BF16)
        if own:
            k.begin_stage()
        cwb = k.sb([128, 12, 5], F32, "cwb")
        k.dma(cwb[:], convwb[0:1536, :].rearrange("(ch p) k -> p ch k", p=128))
        dtb = k.sb([128, 16], F32, "dtb"); k.bcast_row(dtb[:], dt_bias)
        a_b = k.sb([128, 16], F32, "a_b"); k.bcast_row(a_b[:], a_log)
        k.act(a_b[:], a_b[:], AF.Exp)
        k.ts(a_b[:], a_b[:], -1.0, ALU.mult)
        dsk = k.sb([128, 16], F32, "dsk"); k.bcast_row(dsk[:], d_skip)
        nrm = k.sb([128, 1024], F32, "nrm"); k.bcast_row(nrm[:], ssd_norm)
        S = k.sb([128, 1024], F32, "S"); k.memset(S[:], 0.0)
        Sb = k.sb([128, 1024], BF16, "Sb"); k.memset(Sb[:], 0.0)
        xins = k.ring(2, [128, 12, 131], F32, "xin")
        acc = k.sb([128, 12, 128], F32, "acc", gran=128)
        xcvs = k.ring(2, [128, 12, 128], BF16, "xcv")
        xs_tok = k.sb([128, 1024], BF16, "xs_tok")
        bm_tok = k.sb([128, 256], BF16, "bm_tok")
        sm = k.ring(2, [128, 8, 16], F32, "sm")
        dAb = k.sb([128, 16, 128], F32, "dAb")
        cst = k.sb([128, 32], F32, "cst")
        ex = k.sb([128, 3, 16], F32, "ex")
        xdt = k.sb([128, 1024], BF16, "xdt")
        xsk = k.sb([128, 1024], F32, "xsk")
        xw = k.sb([128, 1024], BF16, "xw")
        maskcb = k.sb([128, 2, 128], F32, "maskcb")
        Dms = k.ring(2, [128, 4, 128], F32, "Dm")
        Es = k.ring(2, [128, 4, 128], F32, "E")
        Ms = k.ring(2, [128, 4, 128], BF16, "M")
        y1 = k.sb([128, 1024], F32, "y1")
        zts = k.ring(2, [128, 1024], F32, "zt")
        sz = k.sb([128, 1024], F32, "sz")
        junk = k.sb([128, 512], F32, "junk")
        ssq = k.sb([128, 8], F32, "ssq")
        yas = k.ring(2, [128, 1024], BF16, "ya")
        csb = k.ps([128, 512], F32, "csb")
        tp = csb[:].bitcast(BF16).rearrange("p (a b) -> p a b", a=8)
        Dps = k.ring(1, [128, 4, 128], F32, "Dp", psum=True)
        Y_ps = k.ps([128, 512], F32, "Yps")
        YC = k.ps([128, 512], F32, "YC")

        def b64(ap16):
            return ap16.unsqueeze(2).to_broadcast([128, 16, 64])

        def v64(ap1024):
            return ap1024.rearrange("p (h d) -> p h d", d=64)

        def tile(i):
            xin = xins[i % 2]; xcv = xcvs[i % 2]; s_ = sm[i % 2]
            k.dma(xin[:], cT_s.ap()[0:1536, i * 128:i * 128 + 131].rearrange("(ch p) t -> p ch t", p=128))
            dtr = s_[:, 0, :]
            k.dma(dtr, dt_s.ap()[i * 128:(i + 1) * 128, :])
            zt = zts[i % 2]
            k.dma(zt[:], z_s.ap()[i * 128:(i + 1) * 128, :])
            for ch in range(12):
                k.ts(acc[:, ch, :], xin[:, ch, 3:131], cwb[:, ch, 3:4], ALU.mult, cwb[:, ch, 4:5], ALU.add)
            for kk in range(3):
                for ch in range(12):
                    k.stt(acc[:, ch, :], xin[:, ch, kk:kk + 128], cwb[:, ch, kk:kk + 1], acc[:, ch, :], ALU.mult, ALU.add)
            k.act(xcv[:], acc[:], AF.Silu)
            for c in range(8):
                k.tr(tp[:, c, :], xcv[:, c, :], self.ident[:])
            k.copy(xs_tok[:], tp.rearrange("p c t -> p (c t)"), eng='act')
            for g in range(2):
                k.tr(tp[:, g, :], xcv[:, 8 + g, :], self.ident[:])
            k.copy(bm_tok[:], tp[:, 0:2, :].rearrange("p c t -> p (c t)"), eng='act')
            t1 = s_[:, 1, :]; ab = s_[:, 2, :]; e_ = s_[:, 3, :]; l_ = s_[:, 4, :]; dt = s_[:, 5, :]; dA = s_[:, 6, :]
            k.tt(t1, dtr, dtb[:], ALU.add)
            k.act(ab, t1, AF.Abs)
            k.act(e_, ab, AF.Exp, scale=-1.0)
            k.act(l_, e_, AF.Ln, bias=1.0)
            k.stt(dt, t1, 0.0, l_, ALU.max, ALU.add)
            k.tt(dA, dt, a_b[:], ALU.mult)
            k.copy(dAb[:], dA.unsqueeze(2).to_broadcast([128, 16, 128]), eng='pool')
            k.mm(csb[:, 0:16], self.triu[:], dA)
            k.mm(csb[:, 16:32], self.ones[:], dA)
            k.copy(cst[:], csb[:, 0:32])
            k.act(ex[:, 0, :], cst[:, 0:16], AF.Exp)
            k.act(ex[:, 1, :], cst[:, 16:32], AF.Exp)
            k.tt(s_[:, 7, :], cst[:, 16:32], cst[:, 0:16], ALU.subtract)
            k.act(ex[:, 2, :], s_[:, 7, :], AF.Exp)
            k.tt(v64(xdt[:]), v64(xs_tok[:]), b64(dt), ALU.mult)
            k.tt(v64(xsk[:]), v64(xs_tok[:]), b64(dsk[:]), ALU.mult, eng='pool')
            k.tt(v64(xw[:]), v64(xdt[:]), b64(ex[:, 2, :]), ALU.mult, eng='pool')
            for g in range(2):
                k.mm(csb[:, 128 + g * 128:256 + g * 128], xcv[:, 8 + g, :], xcv[:, 10 + g, :])
            k.tt(maskcb[:], csb[:, 128:384].rearrange("p (g l) -> p g l", g=2),
                 self.triu[:].unsqueeze(1).to_broadcast([128, 2, 128]), ALU.mult)
            def h8(ap512):
                return ap512.rearrange("p (h d) -> p h d", d=64)

            for g in range(2):
                gs = slice(g * 512, (g + 1) * 512)
                for hg in (2 * g, 2 * g + 1):
                    Dp = Dps[0]; Dm = Dms[hg % 2]; E = Es[hg % 2]; M = Ms[hg % 2]
                    for hh in range(4):
                        h = hg * 4 + hh
                        k.mm(Dp[:, hh, :], dAb[:, h, :], self.triu[:], start=True, stop=False)
                        k.mm(Dp[:, hh, :], self.ntriu[:], dAb[:, h, :], start=False, stop=True)
                    k.ts(Dm[:], Dp[:], 0.0, ALU.min)
                    k.act(E[:], Dm[:], AF.Exp)
                    k.tt(M[:], E[:], maskcb[:, g, :].unsqueeze(1).to_broadcast([128, 4, 128]), ALU.mult, eng='pool')
                    for hh in range(4):
                        h = hg * 4 + hh
                        k.mm(Y_ps[:, (h % 8) * 64:(h % 8 + 1) * 64], M[:, hh, :], xdt[:, h * 64:(h + 1) * 64])
                k.mm(YC[:], xcv[:, 10 + g, :], Sb[:, gs])
                k.tt(h8(y1[:, gs]), h8(YC[:]), ex[:, 0, g * 8:(g + 1) * 8].unsqueeze(2).to_broadcast([128, 8, 64]), ALU.mult)
                k.tt(y1[:, gs], y1[:, gs], Y_ps[:], ALU.add)
                k.mm(YC[:], bm_tok[:, g * 128:(g + 1) * 128], xw[:, gs])
                k.tt(h8(S[:, gs]), h8(S[:, gs]), ex[:, 1, g * 8:(g + 1) * 8].unsqueeze(2).to_broadcast([128, 8, 64]), ALU.mult)
                k.tt(S[:, gs], S[:, gs], YC[:], ALU.add)
                k.copy(Sb[:, gs], S[:, gs], eng='act')
            k.tt(y1[:], y1[:], xsk[:], ALU.add)
            k.act(sz[:], zt[:], AF.Silu)
            k.tt(y1[:], y1[:], sz[:], ALU.mult)
            for g in range(2):
                k.act(junk[:], y1[:, g * 512:(g + 1) * 512], AF.Square, accum_out=ssq[:, g:g + 1])
            k.ts(ssq[:, 2:4], ssq[:, 0:2], 1.0 / 512, ALU.mult, EPS, ALU.add)
            k.tt(ssq[:, 6:8], ssq[:, 2:4], self.mhalf[:, 0:2], ALU.pow, eng='pool')
            ya = yas[i % 2]
            for g in range(2):
                k.stt(ya[:, g * 512:(g + 1) * 512], y1[:, g * 512:(g + 1) * 512], ssq[:, 6 + g:7 + g],
                      nrm[:, g * 512:(g + 1) * 512], ALU.mult, ALU.mult)
            k.dma(ya_s.ap()[i * 128:(i + 1) * 128, 0:1024], ya[:], eng='pool')

        if not own:
            return tile
        for i in range(NT):
            tile(i)
        k.end_stage()

    def stage_mlstm(self, convwb, wq, wk, wv, i_bias, f_bias, ml_norm, own=True):
        k = self
        T, NT = self.T, self.NT
        NC = NT; HC = 4 * NC
        cT_s, gT_s, og_s, ya_s = k.scr["cT_s"], k.scr["gT_s"], k.scr["og_s"], k.scr["ya_s"]
        idf, ones, triu = self.idf, self.ones, self.triu
        if own:
            k.begin_stage()
        cwb = k.sb([128, 8, 5], F32, "cwb")
        k.dma(cwb[:], convwb[1536:2560, :].rearrange("(ch p) k -> p ch k", p=128))
        Ws = []
        for nm, w in (("Wq", wq), ("Wk", wk), ("Wv", wv)):
            Wt_ = k.sb([128, 8, 256], BF16, nm)
            k.dma(Wt_[:], w.rearrange("h (dc p) e -> p (h dc) e", p=128), eng='pool')
            Ws.append(Wt_)
        Wq, Wk, Wv = Ws
        nrm = k.sb([128, 1024], F32, "nrm"); k.bcast_row(nrm[:], ml_norm)
        proj = k.ps([128, 1024], F32, "proj")
        p8 = proj[:].rearrange("p (a b) -> p a b", a=8)
        DQ = k.ps([128, 512], F32, "DQ")
        intra = k.ps([128, 512], F32, "intra")
        Dp = DQ
        clp = proj[:].rearrange("p (a b) -> p a b", a=2)
        G = k.sb([HC, 2, 128], F32, "G")
        k.dma(G[:, 0, :], gT_s.ap()[0:4, :].rearrange("h (c l) -> (h c) l", l=128))
        k.dma(G[:, 1, :], gT_s.ap()[4:8, :].rearrange("h (c l) -> (h c) l", l=128))
        bc = k.sb([HC, 2], F32, "bc")
        for h in range(4):
            k.dma(bc[h * NC:(h + 1) * NC, 0:1], i_bias[h:h + 1].partition_broadcast(NC))
            k.dma(bc[h * NC:(h + 1) * NC, 1:2], f_bias[h:h + 1].partition_broadcast(NC))
        pp = k.sb([HC, 12, 128], F32, "prepass")
        xf, ab, e_, l1, logf, it, bcum, u, cmx, mintra, gend, wend = [pp[:, j, :] for j in range(12)]
        pq2 = k.sb([HC, 6, 128], F32, "prepass2")
        mt, t3, at, emt, rowt, _sp = [pq2[:, j, :] for j in range(6)]
        k.ts(xf, G[:, 1, :], bc[:, 1:2], ALU.add)
        k.act(ab, xf, AF.Abs)
        k.act(e_, ab, AF.Exp, scale=-1.0)
        k.act(l1, e_, AF.Ln, bias=1.0)
        k.stt(logf, xf, 0.0, l1, ALU.min, ALU.subtract)
        k.ts(it, G[:, 0, :], bc[:, 0:1], ALU.add)
        k.op('dve', 'tensor_tensor_scan', out=bcum, data0=ones[:HC, :], data1=logf, initial=0.0, op0=ALU.mult, op1=ALU.add)
        k.tt(u, it, bcum, ALU.subtract)
        k.op('dve', 'tensor_tensor_scan', out=cmx, data0=u, data1=u, initial=-1e30, op0=ALU.max, op1=ALU.max)
        k.tt(mintra, bcum, cmx, ALU.add)
        k.ts(gend, u, pp[:, 6, 127:128], ALU.add)
        sc = k.sb([HC, 4], F32, "sc")
        k.op('dve', 'tensor_reduce', out=sc[:, 0:1], in_=gend, axis=AX.X, op=ALU.max)
        k.ts(sc[:, 1:2], sc[:, 0:1], -1.0, ALU.mult)
        k.act(wend, gend, AF.Exp, bias=sc[:, 1:2])
        rp = DQ
        k.tr(rp[0:1, 0:HC], pp[:, 6, 127:128], idf[:HC, :HC])
        k.tr(rp[0:1, 128:128 + HC], sc[:, 0:1], idf[:HC, :HC])
        rows = k.sb([1, 8, 128], F32, "rows")
        k.memset(rows[:], 0.0)
        k.copy(rows[0:1, 0, 0:HC], rp[0:1, 0:HC])
        k.copy(rows[0:1, 1, 0:HC], rp[0:1, 128:128 + HC])
        for h in range(4):
            seg = slice(h * NC, (h + 1) * NC)
            k.op('dve', 'tensor_tensor_scan', out=rows[0:1, 2, seg], data0=rows[0:1, 0, seg], data1=rows[0:1, 1, seg],
                 initial=0.0, op0=ALU.add, op1=ALU.max)
        if NC > 1:
            for h in range(4):
                k.copy(rows[0:1, 3, h * NC + 1:(h + 1) * NC], rows[0:1, 2, h * NC:(h + 1) * NC - 1])
        k.tt(rows[0:1, 4, :], rows[0:1, 0, :], rows[0:1, 3, :], ALU.add)
        k.tt(rows[0:1, 4, :], rows[0:1, 4, :], rows[0:1, 2, :], ALU.subtract)
        k.act(rows[0:1, 5, :], rows[0:1, 4, :], AF.Exp)
        k.tt(rows[0:1, 7, :], rows[0:1, 1, :], rows[0:1, 2, :], ALU.subtract)
        k.act(rows[0:1, 6, :], rows[0:1, 7, :], AF.Exp)
        ABp = intra
        k.mm(ABp[:, 0:256], ones[0:1, :], rows[0:1, 5:7, :].rearrange("p a b -> p (a b)"))
        AB = k.sb([128, 2, 128], F32, "AB")
        k.copy(AB[:], ABp[:, 0:256].rearrange("p (a b) -> p a b", a=2))
        k.mm(rp[0:HC, 256:257], rows[0:1, 3, 0:HC], ones[0:1, 0:1])
        k.copy(sc[:, 2:3], rp[0:HC, 256:257])
        mpc = sc[:, 2:3]
        k.stt(mt, bcum, mpc, mintra, ALU.add, ALU.max)
        k.stt(t3, bcum, mpc, mt, ALU.add, ALU.subtract)
        k.act(at, t3, AF.Exp)
        k.act(emt, mt, AF.Exp, scale=-1.0)
        k.tt(rowt, bcum, mt, ALU.subtract)
        cp2 = proj
        k.tr(cp2[:, 0:HC], at, idf[:HC, :HC])
        k.tr(cp2[:, 128:128 + HC], emt, idf[:HC, :HC])
        k.tr(cp2[:, 256:256 + HC], wend, idf[:HC, :HC])
        cols = k.sb([128, 3, 128], F32, "cols")
        k.memset(cols[:], 0.0)
        k.copy(cols[:, :, 0:HC], cp2[:, 0:384].rearrange("p (a b) -> p a b", a=3)[:, :, 0:HC])
        if own:
            k.barrier()
        C = k.sb([128, 8, 257], F32, "C"); k.memset(C[:], 0.0)
        Cb = k.sb([128, 8, 257], BF16, "Cb"); k.memset(Cb[:], 0.0)
        vext = k.sb([128, 4, 257], BF16, "vext"); k.memset(vext[:], 1.0)
        xins = k.ring(2, [128, 8, 131], F32, "xin")
        acc = k.sb([128, 8, 128], F32, "acc", gran=128)
        xc = k.sb([128, 8, 128], BF16, "xc")
        xmr = k.sb([128, 8, 128], BF16, "xmr")
        ogs = k.ring(2, [128, 1024], F32, "og")
        sg = k.sb([128, 1024], F32, "sg")
        qT = k.sb([128, 8, 128], BF16, "qT")
        kT = k.sb([128, 8, 128], BF16, "kT")
        kw = k.sb([128, 4, 256], BF16, "kw")
        Dms = k.ring(2, [128, 128], F32, "Dm")
        Es = k.ring(2, [128, 128], F32, "E")
        Ems = k.ring(2, [128, 128], F32, "Em")
        Wts = k.ring(2, [128, 128], BF16, "Wt")
        isb = k.sb([128, 257], F32, "isb")
        num = k.sb([128, 257], F32, "num")
        s8 = k.sb([128, 8], F32, "s8")
        junk = k.sb([128, 256], F32, "junk")
        hn_ = k.sb([128, 256], F32, "hn_")
        ybs = k.ring(2, [128, 1024], BF16, "yb")
        def tile(c):
            xin = xins[c % 2]; og = ogs[c % 2]; yb = ybs[c % 2]
            k.dma(xin[:], cT_s.ap()[1536:2560, c * 128:c * 128 + 131].rearrange("(ch p) t -> p ch t", p=128))
            k.dma(og[:], og_s.ap()[c * 128:(c + 1) * 128, :])
            for ch in range(8):
                k.ts(acc[:, ch, :], xin[:, ch, 3:131], cwb[:, ch, 3:4], ALU.mult, cwb[:, ch, 4:5], ALU.add)
            for kk in range(3):
                for ch in range(8):
                    k.stt(acc[:, ch, :], xin[:, ch, kk:kk + 128], cwb[:, ch, kk:kk + 1], acc[:, ch, :], ALU.mult, ALU.add)
            k.act(xc[:], acc[:], AF.Silu)
            k.copy(xmr[:], xin[:, :, 3:131], eng='pool')
            k.act(sg[:], og[:], AF.Sigmoid)
            for (Wm, dst, scl) in ((Wq, qT, None), (Wk, kT, 1.0 / 16)):
                for h in range(4):
                    for ec in range(2):
                        for dc in range(2):
                            k.mm(p8[:, h * 2 + ec, :], Wm[:, h * 2 + dc, ec * 128:(ec + 1) * 128], xc[:, h * 2 + dc, :],
                                 start=(dc == 0), stop=(dc == 1))
                if scl is None:
                    k.copy(dst[:], p8, eng='act')
                else:
                    k.ts(dst[:], p8, scl, ALU.mult)
            for h in range(4):
                for dc in range(2):
                    k.mm(proj[:, h * 256:(h + 1) * 256], xmr[:, h * 2 + dc, :], Wv[:, h * 2 + dc, :], start=(dc == 0), stop=(dc == 1))
            k.copy(vext[:, :, 0:256], proj[:].rearrange("p (h e) -> p h e", h=4), eng='act')
            for h in range(4):
                for dc in range(2):
                    k.mm(proj[:, h * 256:(h + 1) * 256], xc[:, h * 2 + dc, :], Wk[:, h * 2 + dc, :], start=(dc == 0), stop=(dc == 1))
            for h in range(4):
                hc = h * NC + c
                k.ts(kw[:, h, :], proj[:, h * 256:(h + 1) * 256], cols[:, 2, hc:hc + 1], ALU.mult, 1.0 / 16, ALU.mult)
            for h in range(4):
                hc = h * NC + c
                Dm = Dms[h % 2]; E = Es[h % 2]; Em = Ems[h % 2]; Wt = Wts[h % 2]
                sel = idf[:HC, hc:hc + 1].to_broadcast([HC, 128])
                k.mm(Dp[:, 0:128], sel, rowt, start=True, stop=False)
                k.mm(Dp[:, 0:128], u, sel, start=False, stop=True)
                k.ts(Dm[:], Dp[:, 0:128], 0.0, ALU.min)
                k.act(E[:], Dm[:], AF.Exp)
                for ec in range(2):
                    k.mm(DQ[:, 128:256], kT[:, h * 2 + ec, :], qT[:, h * 2 + ec, :], start=(ec == 0), stop=(ec == 1))
                k.tt(Em[:], E[:], triu[:], ALU.mult, eng='pool')
                k.tt(Wt[:], Em[:], DQ[:, 128:256], ALU.mult)
                for dc in range(2):
                    k.mm(DQ[:, 256:512], qT[:, h * 2 + dc, :], Cb[:, h * 2 + dc, 0:256], start=(dc == 0), stop=(dc == 1))
                k.mm(intra[:, 0:257], Wt[:], vext[:, h, :])
                for dc in range(2):
                    k.mm(intra[:, 384:385], qT[:, h * 2 + dc, :], Cb[:, h * 2 + dc, 256:257], start=(dc == 0), stop=(dc == 1))
                k.copy(isb[:], intra[:, 0:257], eng='act')
                k.stt(num[:, 0:256], DQ[:, 256:512], cols[:, 0, hc:hc + 1], isb[:, 0:256], ALU.mult, ALU.add)
                k.stt(num[:, 256:257], intra[:, 384:385], cols[:, 0, hc:hc + 1], isb[:, 256:257], ALU.mult, ALU.add)
                k.act(s8[:, 0:1], num[:, 256:257], AF.Abs)
                k.tt(s8[:, 1:2], s8[:, 0:1], cols[:, 1, hc:hc + 1], ALU.max)
                k.op('dve', 'reciprocal', out=s8[:, 2:3], in_=s8[:, 1:2])
                k.act(junk[:], num[:, 0:256], AF.Square, scale=s8[:, 2:3], accum_out=s8[:, 3:4])
                k.ts(s8[:, 4:5], s8[:, 3:4], 1.0 / 256, ALU.mult, EPS, ALU.add)
                k.tt(s8[:, 6:7], s8[:, 4:5], self.mhalf[:, 0:1], ALU.pow, eng='pool')
                k.tt(s8[:, 7:8], s8[:, 6:7], s8[:, 2:3], ALU.mult)
                k.stt(hn_[:], num[:, 0:256], s8[:, 7:8], nrm[:, h * 256:(h + 1) * 256], ALU.mult, ALU.mult)
                k.tt(yb[:, h * 256:(h + 1) * 256], hn_[:], sg[:, h * 256:(h + 1) * 256], ALU.mult, eng='pool')
                for dc in range(2):
                    k.mm(clp[:, dc, 0:257], kw[:, h, dc * 128:(dc + 1) * 128], vext[:, h, :])
                for dc in range(2):
                    Cs = C[:, h * 2 + dc, :]
                    k.ts(Cs, Cs, AB[:, 0, hc:hc + 1], ALU.mult)
                    k.stt(Cs, clp[:, dc, 0:257], AB[:, 1, hc:hc + 1], Cs, ALU.mult, ALU.add)
                    k.copy(Cb[:, h * 2 + dc, :], Cs, eng='act')
            k.dma(ya_s.ap()[c * 128:(c + 1) * 128, 1024:2048], yb[:], eng='pool')

        if not own:
            return tile
        for c in range(NC):
            tile(c)
        k.end_stage()

    def stage_mix0(self, ssd_args, ml_args):
        k = self
        k.begin_stage()
        ssd_tile = k.stage_ssd(*ssd_args, own=False)
        ml_tile = k.stage_mlstm(*ml_args, own=False)
        k.barrier()
        for i in range(self.NT):
            A = []; B = []
            k.rec = A; ssd_tile(i)
            k.rec = B; ml_tile(i)
            k.rec = None
            k.merge_replay(A, B, grain=getattr(k, 'x_grain', 1))
        k.end_stage()

    def load_w(self, w_ap, KC, N, name):
        W = self.sb([128, KC, N], BF16, name)
        for c in range(KC):
            self.dma(W[:, c, :], w_ap[c * 128:(c + 1) * 128, :], eng='pool')
        return W

    def stage_outproj(self, h_src, y_s, K, w_out, h_dst):
        k = self
        NT = self.NT
        KC = K // 128
        k.begin_stage()
        W = k.load_w(w_out, KC, 1024, "Wout")
        yts = k.ring(2, [128, K], BF16, "yt")
        hts = k.ring(2, [128, 1024], F32, "ht")
        yTs = k.ring(2, [128, KC, 128], BF16, "yT")
        hos = k.ring(2, [128, 1024], F32, "ho")
        tps = k.ring(2, [128, 8, 128], BF16, "tp", psum=True)
        pss = k.ring(4, [128, 512], F32, "pp", psum=True)
        for i in range(NT):
            yt = yts[i % 2]; ht = hts[i % 2]; yT = yTs[i % 2]; ho = hos[i % 2]
            k.dma(yt[:], y_s[i * 128:(i + 1) * 128, :])
            k.dma(ht[:], h_src[i * 128:(i + 1) * 128, :])
            for cg in range(KC // 8):
                tp = tps[cg % 2]
                for c in range(8):
                    k.tr(tp[:, c, :], yt[:, (cg * 8 + c) * 128:(cg * 8 + c + 1) * 128], self.ident[:])
                k.copy(yT[:, cg * 8:(cg + 1) * 8, :], tp[:], eng='act')
            for half in range(2):
                p = pss[(i * 2 + half) % 4]
                for c in range(KC):
                    k.mm(p[:], yT[:, c, :], W[:, c, half * 512:(half + 1) * 512], start=(c == 0), stop=(c == KC - 1))
                k.tt(ho[:, half * 512:(half + 1) * 512], p[:], ht[:, half * 512:(half + 1) * 512], ALU.add)
            k.dma(h_dst[i * 128:(i + 1) * 128, :], ho[:], eng='pool')
        k.end_stage()

    def merge_replay(self, A, B, grain=3):
        na, nb = len(A), len(B)
        steps = max(1, min(na, nb) // grain)
        for j in range(steps):
            self.replay(A[j * na // steps:(j + 1) * na // steps])
            self.replay(B[j * nb // steps:(j + 1) * nb // steps])

    def stage_xattn(self, h_src, h_dst, mem, g_x, g_m, wq, wkv, wo):
        k = self
        NT = self.NT
        ident = self.ident
        k.begin_stage()
        Wq = k.load_w(wq, 8, 1024, "Wq")
        Wo = k.load_w(wo, 8, 1024, "Wo")
        Wkv = k.load_w(wkv, 8, 2048, "Wkv")
        gx = k.sb([128, D], F32, "gx"); k.bcast_row(gx[:], g_x)
        gm = k.sb([128, D], F32, "gm"); k.bcast_row(gm[:], g_m)
        memT = k.sb([128, 8, 256], BF16, "memT")
        kT = k.sb([128, 8, 256], BF16, "kT")
        vx = k.sb([128, 2, 1024], BF16, "vx")

        def make_stream(sid):
            ht = k.sb([128, D], F32, "ht")
            ss = k.sb([128, 4], F32, "ss")
            hn = k.sb([128, D], BF16, "hn")
            hT = k.sb([128, 8, 128], BF16, "hT")
            qT = k.sb([128, 8, 128], BF16, "qT")
            Pm = k.sb([128, 4, 256], BF16, "Pm")
            s_ = k.sb([128, 16], F32, "s16")
            PTs = k.sb([128, 8, 128], BF16, "PTs")
            osb = k.sb([128, 1024], BF16, "osb")
            oT = k.sb([128, 8, 128], BF16, "oT")
            ho = k.sb([128, D], F32, "ho")
            bT = k.ps([128, 512], F32, "bT")
            bA = k.ps([128, 512], F32, "bA")
            bB = k.ps([128, 1024], F32, "bB")
            tp = bT[:].bitcast(BF16).rearrange("p (a b) -> p a b", a=8)
            bA4 = bA[:].rearrange("p (a b) -> p a b", a=4)
            bB4 = bB[:].rearrange("p (a b) -> p a b", a=4)
            st = dict(ht=ht, ss=ss, hn=hn, tp=tp, bA=bA, bB=bB)

            def tile(i):
                k.dma(ht[:], h_src[i * 128:(i + 1) * 128, :])
                k.rms_T(ht, gx, hn, hT[:], tp, ss, hn)
                for half in range(2):
                    for j4 in range(4):
                        j = half * 4 + j4
                        for c in range(8):
                            k.mm(bA4[:, j4, :], Wq[:, c, j * 128:(j + 1) * 128], hT[:, c, :], start=(c == 0), stop=(c == 7))
                    k.ts(qT[:, half * 4:(half + 1) * 4, :], bA4, 1.0 / 16, ALU.mult)
                for h in range(4):
                    for dc in range(2):
                        k.mm(bB4[:, h, :], qT[:, h * 2 + dc, :], kT[:, h * 2 + dc, :], start=(dc == 0), stop=(dc == 1))
                k.op('dve', 'tensor_reduce', out=s_[:, 0:4], in_=bB4, axis=AX.X, op=ALU.max)
                k.ts(s_[:, 4:8], s_[:, 0:4], -1.0, ALU.mult)
                for h in range(4):
                    k.act(Pm[:, h, :], bB4[:, h, :], AF.Exp, bias=s_[:, 4 + h:5 + h], accum_out=s_[:, 8 + h:9 + h])
                k.op('dve', 'reciprocal', out=s_[:, 12:16], in_=s_[:, 8:12])
                for h in range(4):
                    for mc in range(2):
                        k.tr(tp[:, h * 2 + mc, :], Pm[:, h, mc * 128:(mc + 1) * 128], ident[:])
                k.copy(PTs[:], tp, eng='act')
                for half in range(2):
                    for h2 in range(2):
                        h = half * 2 + h2
                        for mc in range(2):
                            k.mm(bA[:, h2 * 256:(h2 + 1) * 256], PTs[:, h * 2 + mc, :], vx[:, mc, h * 256:(h + 1) * 256],
                                 start=(mc == 0), stop=(mc == 1))
                    k.tt(osb[:, half * 512:(half + 1) * 512].rearrange("p (h e) -> p h e", h=2),
                         bA[:].rearrange("p (h e) -> p h e", h=2),
                         s_[:, 12 + half * 2:14 + half * 2].unsqueeze(2).to_broadcast([128, 2, 256]), ALU.mult)
                for c in range(8):
                    k.tr(tp[:, c, :], osb[:, c * 128:(c + 1) * 128], ident[:])
                k.copy(oT[:], tp, eng='act')
                for half in range(2):
                    reg = bB[:, half * 512:(half + 1) * 512]
                    for c in range(8):
                        k.mm(reg, oT[:, c, :], Wo[:, c, half * 512:(half + 1) * 512], start=(c == 0), stop=(c == 7))
                k.tt(ho[:], bB[:], ht[:], ALU.add)
                k.dma(h_dst[i * 128:(i + 1) * 128, :], ho[:], eng='pool')
            return tile, st

        t0, st0 = make_stream(0)
        t1, st1 = make_stream(1)
        for mc in range(2):
            k.dma(st0['ht'][:], mem[mc * 128:(mc + 1) * 128, :])
            k.rms_T(st0['ht'], gm, st0['hn'], memT[:, :, mc * 128:(mc + 1) * 128], st0['tp'], st0['ss'], st0['hn'])
        for j in range(8):
            reg = st0['bB'][:, (j % 4) * 256:(j % 4 + 1) * 256]
            for c in range(8):
                k.mm(reg, Wkv[:, c, j * 128:(j + 1) * 128], memT[:, c, :], start=(c == 0), stop=(c == 7))
            k.copy(kT[:, j, :], reg, eng='act')
        for mc in range(2):
            for half in range(2):
                reg = st1['bB'][:, half * 512:(half + 1) * 512]
                for c in range(8):
                    k.mm(reg, memT[:, c, mc * 128:(mc + 1) * 128], Wkv[:, c, 1024 + half * 512:1024 + (half + 1) * 512],
                         start=(c == 0), stop=(c == 7))
                k.copy(vx[:, mc, half * 512:(half + 1) * 512], reg)
        if NT % 2 == 0 and NT >= 2:
            H2 = NT // 2
            for i in range(H2):
                A = []; B = []
                k.rec = A; t0(i)
                k.rec = B; t1(i + H2)
                k.rec = None
                k.merge_replay(A, B, grain=getattr(k, 'x_grain', 1))
        else:
            for i in range(NT):
                t0(i)
        k.end_stage()

    def precast_ops(self, pairs):
        k = self
        st = k.ring(3, [128, 2, 1024], F32, "pc32")
        sb = k.ring(3, [128, 2, 1024], BF16, "pc16")
        ops = []
        k.rec = ops
        n = 0
        for (src, dst) in pairs:
            R_ = src.shape[0]
            for r0 in range(0, R_, 256):
                a = st[n % 3]; b = sb[n % 3]
                k.dma(a[:], src[r0:r0 + 256, :].rearrange("(p j) n -> p j n", j=2))
                eng = ('dve', 'act', 'dve', 'pool')[n % 4]
                k.copy(b[:], a[:], eng=eng)
                k.dma(dst[r0:r0 + 256, :].rearrange("(p j) n -> p j n", j=2), b[:], eng='pool')
                n += 1
        k.rec = None
        return ops

    def stage_precast(self, pairs):
        k = self
        k.begin_stage()
        k.replay(k.precast_ops(pairs))
        k.end_stage()

    def stage_peer(self, h_src, h_dst, g_f, wq, skT, uR, vtab, final_gamma=None):
        k = self
        T = self.T
        TP = 256 if T >= 256 else 128
        NS = TP // 128
        NTP = T // TP
        idf, ident = self.idf, self.ident
        self._uid += 1
        gtd = [[k.dram("gtd%d_%d_%d" % (j, ig, self._uid), [16, 128, TP], BF16).ap() for ig in range(8)]
               for j in range(2)]
        k.begin_stage()
        Wq = k.load_w(wq, 8, 2048, "Wpq")
        sk = k.sb([128, 16, 128], BF16, "sk")
        k.dma(sk[:], skT, eng='pool')
        gf = k.sb([128, D], F32, "gf"); k.bcast_row(gf[:], g_f)
        if final_gamma is not None:
            gfin = k.sb([128, D], F32, "gfin"); k.bcast_row(gfin[:], final_gamma)
        ioti = k.sb([128, 128], I32, "ioti")
        k.op('pool', 'iota', out=ioti[:], pattern=[[1, 128]], base=0, channel_multiplier=0)
        iotf = k.sb([128, 128], F32, "iotf")
        k.copy(iotf[:], ioti[:])
        iotb = k.sb([128, 128], BF16, "iotb")
        k.copy(iotb[:], ioti[:])
        GT = k.sb([128, 128, TP], BF16, "GT", gran=16 * TP)
        xTs = k.ring(2, [128, 8, TP], BF16, "xT")
        qT = k.sb([128, 16, TP], BF16, "qT", gran=TP)
        hts = k.ring(2, [128, D], F32, "ht")
        hn = k.sb([128, D], BF16, "hn")
        ss = k.sb([128, 4], F32, "ss")
        ss2 = k.sb([128, 4], F32, "ss2")
        V = k.sb([128, 16, 16], F32, "V", gran=8)
        IX = k.sb([128, 16, 16], U32, "IX", gran=8)
        IXf = k.sb([128, 16, 16], F32, "IXf")
        wk = k.sb([128, 2, 128], F32, "wk", gran=128)
        cand = k.sb([128, 8, 16, 16], F32, "cand", gran=256)
        eq = cand
        wk2 = k.sb([128, 2, 256], F32, "wk2", gran=256)
        TSs = k.ring(2, [128, 8, 16], F32, "TS", gran=8)
        PX = k.sb([128, 8, 16], U32, "PX", gran=8)
        PAB = k.sb([128, 2, 8, 16], U32, "PAB")
        PABf = k.sb([128, 2, 8, 16], F32, "PABf")
        I12Ws = k.ring(2, [128, 3, 128], F32, "I12W", gran=128)
        sgs = k.ring(2, [128, 32], F32, "sg", gran=1)
        Eg = k.sb([128, 8, 16], F32, "Eg", gran=16)
        IT = k.sb([128, 3, TP], F32, "IT")
        Pms = k.sb([128, 16, 128], BF16, "Pm", gran=128)
        Qms = k.sb([128, 16, 128], BF16, "Qm", gran=128)
        NR = 3
        urs = k.ring(NR, [128, 2, 1024], BF16, "ur")
        vrs = k.ring(NR, [128, 2, 1024], BF16, "vr")
        gtr = k.ring(NR, [128, 2, TP], BF16, "gtr")
        gls = k.ring(3, [128, TP], F32, "gl")
        GAs = k.ring(3, [128, TP], BF16, "GA")
        hos = k.ring(1, [128, D], F32, "ho")
        big4 = k.ps([128, 4, 512], F32, "big4")
        r2 = k.ps([128, 2, 512], F32, "r2")
        ab = k.ring(2, [128, 512], F32, "ab", psum=True)
        r2f = r2[:].rearrange("p a b -> p (a b)")
        Vv = V[:].rearrange("p (h q) k -> p h q k", q=2)
        IXv = IXf[:].rearrange("p (h q) k -> p h q k", q=2)
        B4 = [128, 8, 16, 16]
        cnt = {'r2': 0, 'ab': 0}

        def r2s(width):
            j = cnt['r2'] % 4; cnt['r2'] += 1
            return r2f[:, j * 256:j * 256 + width]

        def abn():
            j = cnt['ab'] % 2; cnt['ab'] += 1
            return ab[j]

        def phase_ab(n):
            t0 = n * TP
            xT = xTs[n % 2]
            for st in range(NS):
                ht = hts[st % 2]
                k.dma(ht[:], h_src[t0 + st * 128:t0 + (st + 1) * 128, :])
                tpb = abn()[:].bitcast(BF16).rearrange("p (a b) -> p a b", a=8)
                k.act(hn[:], ht[:], AF.Square, accum_out=ss[:, 0:1])
                k.ts(ss[:, 1:2], ss[:, 0:1], 1.0 / D, ALU.mult, EPS, ALU.add)
                k.tt(ss[:, 3:4], ss[:, 1:2], self.mhalf[:, 0:1], ALU.pow, eng='pool')
                k.stt(hn[:], ht[:], ss[:, 3:4], gf[:], ALU.mult, ALU.mult)
                for c in range(8):
                    k.tr(tpb[:, c, :], hn[:, c * 128:(c + 1) * 128], ident[:])
                k.copy(xT[:, :, st * 128:(st + 1) * 128], tpb, eng='act')
            for hp in range(16):
                p = abn()[:, 0:TP]
                for c in range(8):
                    k.mm(p, Wq[:, c, hp * 128:(hp + 1) * 128], xT[:, c, :], start=(c == 0), stop=(c == 7))
                k.copy(qT[:, hp, :], p, eng=('act' if hp % 2 else 'dve'))
            def it_transposes(st):
                I12 = I12Ws[st % 2]
                p = abn()
                for j in range(3):
                    k.tr(p[:, j * 128:(j + 1) * 128], I12[:, j, :], idf[:])
                k.copy(IT[:, :, st * 128:(st + 1) * 128], p[:, 0:384].rearrange("p (a b) -> p a b", a=3), eng='act')

            def gbuild(ta, tb_):
                NG = (tb_ - ta) // 4
                p4s = {}
                for it in range(NG + 3):
                    g_ts = it; g_mm = it - 2; g_cp = it - 3
                    if g_ts < NG:
                        for tt_ in range(4):
                            t = ta + g_ts * 4 + tt_
                            sl_ = t % 16
                            k.ts(Pms[:, sl_, :], iotb[:], IT[:, 0, t:t + 1], ALU.is_equal, eng=('pool' if t % self.x_pool_every == 0 else 'dve'))
                            k.ts(Qms[:, sl_, :], iotb[:], IT[:, 1, t:t + 1], ALU.is_equal, IT[:, 2, t:t + 1], ALU.mult)
                    if 0 <= g_mm < NG:
                        p4 = abn()[:].rearrange("p (a b) -> p a b", a=4)
                        p4s[g_mm] = p4
                        for tt_ in range(4):
                            sl_ = (ta + g_mm * 4 + tt_) % 16
                            k.mm(p4[:, tt_, :], Qms[:, sl_, :], Pms[:, sl_, :])
                    if 0 <= g_cp < NG:
                        tq = ta + g_cp * 4
                        k.copy(GT[:, :, tq:tq + 4], p4s.pop(g_cp).rearrange("p t i -> p i t"),
                               eng=('dve' if g_cp % 4 == 0 else 'act'))

            def gate(st):
                I12W = I12Ws[st % 2]; TS = TSs[st % 2]; sg = sgs[st % 2]
                k.ts(sg[:, 0:8], TS[:, :, 0], -1.0, ALU.mult)
                for h in range(8):
                    k.act(Eg[:, h, :], TS[:, h, :], AF.Exp, bias=sg[:, h:h + 1], accum_out=sg[:, 8 + h:9 + h])
                k.op('dve', 'reciprocal', out=sg[:, 16:24], in_=sg[:, 8:16])
                k.tt(I12W[:, 2, :].rearrange("p (h k) -> p h k", h=8), Eg[:], sg[:, 16:24].unsqueeze(2).to_broadcast([128, 8, 16]), ALU.mult)

            for st in range(NS):
                I12W = I12Ws[st % 2]; TS = TSs[st % 2]
                for g0 in range(0, 16, 4):
                    hps = range(g0, g0 + 4)
                    S4 = abn()[:].rearrange("p (a b) -> p a b", a=4)
                    for hp in hps:
                        k.mm(S4[:, hp % 4, :], qT[:, hp, st * 128:(st + 1) * 128], sk[:, hp, :])
                    for hps2 in (range(g0, g0 + 2), range(g0 + 2, g0 + 4)):
                        for hp in hps2:
                            k.op('dve', 'max', out=V[:, hp, 0:8], in_=S4[:, hp % 4, :])
                        for hp in hps2:
                            k.op('dve', 'max_index', out=IX[:, hp, 0:8], in_max=V[:, hp, 0:8], in_values=S4[:, hp % 4, :])
                        for hp in hps2:
                            k.op('dve', 'match_replace', out=wk[:, hp % 2, :], in_to_replace=V[:, hp, 0:8], in_values=S4[:, hp % 4, :], imm_value=-1e30)
                        for hp in hps2:
                            k.op('dve', 'max', out=V[:, hp, 8:16], in_=wk[:, hp % 2, :])
                        for hp in hps2:
                            k.op('dve', 'max_index', out=IX[:, hp, 8:16], in_max=V[:, hp, 8:16], in_values=wk[:, hp % 2, :])
                k.copy(IXf[:], IX[:])
                k.tt(cand[:], Vv[:, :, 0, :].unsqueeze(3).to_broadcast(B4), Vv[:, :, 1, :].unsqueeze(2).to_broadcast(B4), ALU.add)
                cvs = [cand[:, h, :, :].rearrange("p a b -> p (a b)") for h in range(8)]
                for g0 in range(0, 8, 2):
                    hsr = range(g0, g0 + 2)
                    for h in hsr:
                        k.op('dve', 'max', out=TS[:, h, 0:8], in_=cvs[h])
                    for h in hsr:
                        k.op('dve', 'max_index', out=PX[:, h, 0:8], in_max=TS[:, h, 0:8], in_values=cvs[h])
                    for h in hsr:
                        k.op('dve', 'match_replace', out=wk2[:, h % 2, :], in_to_replace=TS[:, h, 0:8], in_values=cvs[h], imm_value=-1e30)
                    for h in hsr:
                        k.op('dve', 'max', out=TS[:, h, 8:16], in_=wk2[:, h % 2, :])
                    for h in hsr:
                        k.op('dve', 'max_index', out=PX[:, h, 8:16], in_max=TS[:, h, 8:16], in_values=wk2[:, h % 2, :])
                k.op('dve', 'tensor_single_scalar', out=PAB[:, 0, :, :], in_=PX[:], scalar=4, op=ALU.logical_shift_right)
                k.op('dve', 'tensor_single_scalar', out=PAB[:, 1, :, :], in_=PX[:], scalar=15, op=ALU.bitwise_and)
                k.copy(PABf[:], PAB[:])
                for q in range(2):
                    k.tt(eq[:], iotf[:, 0:16].unsqueeze(1).unsqueeze(1).to_broadcast(B4),
                         PABf[:, q, :, :].unsqueeze(3).to_broadcast(B4), ALU.is_equal)
                    k.tt(eq[:], eq[:], IXv[:, :, q, :].unsqueeze(2).to_broadcast(B4), ALU.mult)
                    k.op('dve', 'tensor_reduce', out=I12W[:, q, :].rearrange("p (h k) -> p h k", h=8), in_=eq[:], axis=AX.X, op=ALU.add)
            if k.rec is not None:
                k.rec.append(('marker', 34, None, 0))
            for st in range(NS):
                gate(st)
                it_transposes(st)
                gbuild(st * 128, (st + 1) * 128)
            for ig in range(2, 8):
                k.dma(gtd[n % 2][ig].rearrange("i j t -> j i t"), GT[:, ig * 16:(ig + 1) * 16, :], eng='act')

        def dense(n, side):
            xT = xTs[n % 2]
            g_ = gtd[n % 2]

            def load(i):
                sl_ = (i // 2) % NR
                k.dma(urs[sl_][:], uR[i:i + 2].rearrange("i p n -> p i n"))
                k.dma(vrs[sl_][:], vtab[i * 128:(i + 2) * 128, :].rearrange("(i p) n -> p i n", p=128))
                if i >= 32:
                    k.dma(gtr[sl_][:], g_[i // 16][i % 16:i % 16 + 2].rearrange("i j t -> j i t"))

            def uphase(i):
                sl_ = (i // 2) % NR
                p = r2s(TP)
                for c in range(8):
                    k.mm(p, urs[sl_][:, i % 2, c * 128:(c + 1) * 128], xT[:, c, :], start=(c == 0), stop=(c == 7))
                gl = gls[i % 3]; GA = GAs[i % 3]
                k.act(gl[:], p, AF.Gelu)
                k.tt(GA[:], gl[:], (GT[:, i, :] if i < 32 else gtr[sl_][:, i % 2, :]), ALU.mult, eng='pool')

            def vphase(i):
                vr = vrs[(i // 2) % NR]; GA = GAs[i % 3]
                for st in range(NS):
                    for dh in range(2):
                        k.mm(big4[:, st * 2 + dh, :], GA[:, st * 128:(st + 1) * 128], vr[:, i % 2, dh * 512:(dh + 1) * 512],
                             start=(i == 0), stop=(i == 127))

            tot_cost = sum(o[3] for o in side)
            per_cost = tot_cost / 100.0 + 1
            for i in range(0, 2 * (NR - 1), 2):
                load(i)
            uphase(0)
            for i in range(128):
                if i % 2 == 0 and i + 2 * (NR - 1) < 128:
                    load(i + 2 * (NR - 1))
                if i + 1 < 128:
                    uphase(i + 1)
                vphase(i)
                side = k.replay_cost(side, per_cost, chunk=i)
            k.replay(side)
            t0 = n * TP
            for st in range(NS):
                ho = hos[0]; ht = hts[st % 2]
                k.dma(ht[:], h_src[t0 + st * 128:t0 + (st + 1) * 128, :])
                k.tt(ho[:], big4[:, st * 2:st * 2 + 2, :].rearrange("p a b -> p (a b)"), ht[:], ALU.add)
                if final_gamma is not None:
                    k.act(hn[:], ho[:], AF.Square, accum_out=ss2[:, 0:1])
                    k.ts(ss2[:, 1:2], ss2[:, 0:1], 1.0 / D, ALU.mult, EPS, ALU.add)
                    k.tt(ss2[:, 3:4], ss2[:, 1:2], self.mhalf[:, 0:1], ALU.pow, eng='pool')
                    k.stt(ho[:], ho[:], ss2[:, 3:4], gfin[:], ALU.mult, ALU.mult)
                k.dma(h_dst[t0 + st * 128:t0 + (st + 1) * 128, :], ho[:], eng='pool')

        phase_ab(0)
        for n in range(NTP):
            side = []
            if n + 1 < NTP:
                k.rec = side
                phase_ab(n + 1)
                k.rec = None
            dense(n, side)
        k.end_stage()

    def stage_hgrn(self, h_src, h_dst, gamma, w_in, w_out, lb_logits, hnorm):
        k = self
        NT = self.NT
        ident, triu, ones = self.ident, self.triu, self.ones
        k.begin_stage()
        Win = k.load_w(w_in, 8, 4096, "Win")
        Wout = k.load_w(w_out, 8, 1024, "Wout")
        gb = k.sb([128, D], F32, "gb"); k.bcast_row(gb[:], gamma)
        hnb = k.sb([128, D], F32, "hnb"); k.bcast_row(hnb[:], hnorm)
        lb = k.sb([128, D], F32, "lb"); k.bcast_row(lb[:], lb_logits[0])
        oml = k.sb([128, D], F32, "oml"); k.bcast_row(oml[:], lb_logits[1])
        k.tt(lb[:], lb[:], oml[:], ALU.subtract)
        k.act(lb[:], lb[:], AF.Sigmoid)
        k.ts(oml[:], lb[:], -1.0, ALU.mult, 1.0, ALU.add)
        A1 = k.sb([128, 128], F32, "A1")
        k.op('pool', 'affine_select', out=A1[:], in_=ones[:], pattern=[[0, 128]], compare_op=ALU.is_ge, fill=0.0,
             base=64, channel_multiplier=-1)
        k.tt(A1[:], triu[:], A1[:], ALU.subtract)
        A3 = k.sb([128, 128], F32, "A3")
        k.tt(A3[:], ones[:], triu[:], ALU.subtract)
        S = k.sb([128, 8, 128], F32, "S"); k.memset(S[:], 0.0)
        Sb = k.sb([128, 8, 128], BF16, "Sb"); k.memset(Sb[:], 0.0)
        hts = k.ring(2, [128, D], F32, "ht")
        ss = k.sb([128, 4], F32, "ss")
        hn = k.sb([128, D], BF16, "hn")
        hTs = k.ring(2, [128, 8, 128], BF16, "hT")
        qs = k.ring(2, [128, D], F32, "q")
        fs = k.ring(2, [128, D], F32, "f")
        sgts = k.ring(2, [128, D], F32, "sgt")
        vs = k.ring(2, [128, D], BF16, "v")
        logf = k.sb([128, D], F32, "logf")
        kk = k.sb([128, D], F32, "kk")
        ex = k.sb([128, D], F32, "ex")
        qa = k.sb([128, D], BF16, "qa"); ka = k.sb([128, D], BF16, "ka")
        qg = k.sb([128, D], BF16, "qg"); kl = k.sb([128, D], BF16, "kl")
        qaT = k.sb([128, 8, 128], BF16, "qaT"); kaT = k.sb([128, 8, 128], BF16, "kaT"); qgT = k.sb([128, 8, 128], BF16, "qgT")
        cd = k.sb([128, 8], F32, "cd")
        attm4s = k.ring(2, [128, 4, 128], BF16, "attm4")
        o1 = k.sb([128, D], F32, "o1")
        s8 = k.sb([128, 32], F32, "s8")
        on = k.sb([128, D], BF16, "on")
        oT = k.sb([128, 8, 128], BF16, "oT")
        hos = k.ring(2, [128, D], F32, "ho")
        prA = k.ps([128, 512], F32, "prA")
        tpAf = k.ps([128, 512], F32, "tpA")
        tpA = tpAf[:].bitcast(BF16).rearrange("p (a b) -> p a b", a=8)
        pG = k.ps([128, 2, 512], F32, "pG")
        tp = k.ps([128, 8, 128], BF16, "tp")
        attp = k.ps([128, 512], F32, "attp")
        o_ps = k.ps([128, 1024], F32, "o_ps")
        sl = pG[:].rearrange("p a (b c) -> p (a b) c", c=128)

        def v8(ap):
            return ap.rearrange("p (h d) -> p h d", h=8)

        def prefix(i):
            ht = hts[i % 2]; hT = hTs[i % 2]
            q = qs[i % 2]; f = fs[i % 2]; v = vs[i % 2]; sgt = sgts[i % 2]
            k.dma(ht[:], h_src[i * 128:(i + 1) * 128, :])
            k.rms_T(ht, gb, hn, hT[:], tpA, ss, hn)

            def proj(col0, fn):
                for half in range(2):
                    for c in range(8):
                        k.mm(prA[:], hT[:, c, :], Win[:, c, col0 + half * 512:col0 + (half + 1) * 512], start=(c == 0), stop=(c == 7))
                    fn(prA, slice(half * 512, (half + 1) * 512))
            proj(0, lambda p, sl_: k.act(q[:, sl_], p[:], AF.Silu))
            proj(3072, lambda p, sl_: k.act(sgt[:, sl_], p[:], AF.Silu))
            proj(1024, lambda p, sl_: k.act(f[:, sl_], p[:], AF.Sigmoid))
            proj(2048, lambda p, sl_: k.copy(v[:, sl_], p[:]))

        def rest(i):
            ht = hts[i % 2]; ho = hos[i % 2]
            q = qs[i % 2]; f = fs[i % 2]; v = vs[i % 2]; sgt = sgts[i % 2]
            k.tt(f[:], f[:], oml[:], ALU.mult)
            k.tt(f[:], f[:], lb[:], ALU.add)
            k.act(logf[:], f[:], AF.Ln)
            k.ts(kk[:], f[:], -1.0, ALU.mult, 1.0, ALU.add)
            pGf = pG[:].rearrange("p a b -> p (a b)")
            for half in range(2):
                k.mm(pG[:, half, :], A1[:], logf[:, half * 512:(half + 1) * 512])
            k.act(ex[:], pGf, AF.Exp)
            k.tt(qa[:], q[:], ex[:], ALU.mult)
            k.act(ex[:], pGf, AF.Exp, scale=-1.0)
            k.tt(ka[:], kk[:], ex[:], ALU.mult)
            for half in range(2):
                k.mm(pG[:, half, :], triu[:], logf[:, half * 512:(half + 1) * 512])
            k.act(ex[:], pGf, AF.Exp)
            k.tt(qg[:], q[:], ex[:], ALU.mult)
            for half in range(2):
                k.mm(pG[:, half, :], A3[:], logf[:, half * 512:(half + 1) * 512])
            k.act(ex[:], pGf, AF.Exp)
            k.tt(kl[:], kk[:], ex[:], ALU.mult)
            for h in range(8):
                k.mm(attp[:, 256 + h:257 + h], logf[:, h * 128:(h + 1) * 128], ones[:, 0:1])
            k.act(cd[:], attp[:, 256:264], AF.Exp)
            for (src, dst) in ((qa, qaT), (ka, kaT), (qg, qgT)):
                for c in range(8):
                    k.tr(tp[:, c, :], src[:, c * 128:(c + 1) * 128], ident[:])
                k.copy(dst[:], tp[:], eng='act')
            attp4 = attp[:].rearrange("p (a b) -> p a b", a=4)
            for hq in range(2):
                attm4 = attm4s[hq]
                for h4 in range(4):
                    h = hq * 4 + h4
                    k.mm(attp4[:, h4, :], kaT[:, h, :], qaT[:, h, :])
                k.tt(attm4[:], attp4, triu[:].unsqueeze(1).to_broadcast([128, 4, 128]), ALU.mult)
                for h4 in range(4):
                    h = hq * 4 + h4
                    hs = slice(h * 128, (h + 1) * 128)
                    k.mm(o_ps[:, hs], attm4[:, h4, :], v[:, hs], start=True, stop=False)
                    k.mm(o_ps[:, hs], qgT[:, h, :], Sb[:, h, :], start=False, stop=True)
                    k.mm(sl[:, h, :], kl[:, hs], v[:, hs])
            for h in range(8):
                k.stt(S[:, h, :], S[:, h, :], cd[:, h:h + 1], sl[:, h, :], ALU.mult, ALU.add)
            k.copy(Sb[:], S[:], eng='act')
            k.act(ex[:], o_ps[:], AF.Square)
            k.op('dve', 'tensor_reduce', out=s8[:, 0:8], in_=v8(ex[:]), axis=AX.X, op=ALU.add)
            k.ts(s8[:, 8:16], s8[:, 0:8], 1.0 / 128, ALU.mult, EPS, ALU.add)
            k.tt(s8[:, 24:32], s8[:, 8:16], self.mhalf[:, 0:8], ALU.pow, eng='pool')
            k.tt(v8(o1[:]), v8(o_ps[:]), s8[:, 24:32].unsqueeze(2).to_broadcast([128, 8, 128]), ALU.mult)
            k.tt(o1[:], o1[:], hnb[:], ALU.mult, eng='pool')
            k.tt(on[:], o1[:], sgt[:], ALU.mult)
            for c in range(8):
                k.tr(tp[:, c, :], on[:, c * 128:(c + 1) * 128], ident[:])
            k.copy(oT[:], tp[:], eng='act')
            for half in range(2):
                for c in range(8):
                    k.mm(pG[:, half, :], oT[:, c, :], Wout[:, c, half * 512:(half + 1) * 512], start=(c == 0), stop=(c == 7))
                k.tt(ho[:, half * 512:(half + 1) * 512], pG[:, half, :], ht[:, half * 512:(half + 1) * 512], ALU.add)
            k.dma(h_dst[i * 128:(i + 1) * 128, :], ho[:], eng='pool')

        prefix(0)
        for i in range(NT):
            A = []; B = []
            k.rec = A; rest(i)
            if i + 1 < NT:
                k.rec = B; prefix(i + 1)
            k.rec = None
            if B:
                k.merge_replay(A, B, grain=getattr(k, 'x_grain', 1))
            else:
                k.replay(A)
        k.end_stage()


def build_full(T=4096, dbg=()):
    k = MK(T, dbg=dbg)
    I = {}
    def inp(name, shape, dt=F32):
        I[name] = k.inp(name, shape, dt).ap()
        return I[name]
    inp("x", [T, 1024]); inp("mem", [256, 1024])
    for n in ("norm_mix", "norm_xattn", "norm_mem", "norm_ffn", "hgrn_lb_logits"):
        inp(n, [2, 1024])
    inp("norm_final", [1024])
    inp("ev_w_in", [1024, E_IN]); inp("ev_w_out", [2048, 1024]); inp("convwb", [2560, 5])
    for n in ("ssd_dt_bias", "ssd_a_log", "ssd_d_skip"):
        inp(n, [16])
    inp("ssd_norm", [1024])
    for n in ("ml_wq", "ml_wk", "ml_wv"):
        inp(n, [4, 256, 256])
    inp("ml_i_bias", [4]); inp("ml_f_bias", [4]); inp("ml_norm", [1024])
    inp("od_w_in", [1024, 4096]); inp("od_w_out", [1024, 1024]); inp("hgrn_norm", [1024])
    inp("xa_wq", [2, 1024, 1024]); inp("xa_wkv", [2, 1024, 2048]); inp("xa_wo", [2, 1024, 1024])
    inp("peer_wq", [2, 1024, 2048]); inp("peer_skT", [2, 128, 16, 128])
    inp("peer_uR", [2, 16384, 1024]); inp("peer_v", [2, 16384, 1024])
    out = k.dram("out", [T, 1024], F32, kind="ExternalOutput").ap()
    k.make_consts()
    ub = [k.scratch("ub%d_s" % l, [16384, 1024], BF16).ap() for l in range(2)]
    vb = [k.scratch("vb%d_s" % l, [16384, 1024], BF16).ap() for l in range(2)]
    hs = [k.scratch("h%d_s" % j, [T, 1024]).ap() for j in range(1, 6)]
    pairs = [(I["peer_uR"][l], ub[l]) for l in range(2)] + [(I["peer_v"][l], vb[l]) for l in range(2)]
    k.stage_inproj_even(I["x"], I["ev_w_in"], I["norm_mix"][0], side_fn=lambda: k.precast_ops(pairs))
    k.stage_mix0((I["convwb"], I["ssd_dt_bias"], I["ssd_a_log"], I["ssd_d_skip"], I["ssd_norm"]),
                 (I["convwb"], I["ml_wq"], I["ml_wk"], I["ml_wv"], I["ml_i_bias"], I["ml_f_bias"], I["ml_norm"]))
    k.stage_outproj(I["x"], k.scr["ya_s"].ap(), 2048, I["ev_w_out"], hs[0])
    k.stage_xattn(hs[0], hs[1], I["mem"], I["norm_xattn"][0], I["norm_mem"][0], I["xa_wq"][0], I["xa_wkv"][0], I["xa_wo"][0])
    k.stage_peer(hs[1], hs[2], I["norm_ffn"][0], I["peer_wq"][0], I["peer_skT"][0],
                 ub[0].rearrange("(i p) n -> i p n", p=128), vb[0])
    k.stage_hgrn(hs[2], hs[3], I["norm_mix"][1], I["od_w_in"], I["od_w_out"], I["hgrn_lb_logits"], I["hgrn_norm"])
    k.stage_xattn(hs[3], hs[4], I["mem"], I["norm_xattn"][1], I["norm_mem"][1], I["xa_wq"][1], I["xa_wkv"][1], I["xa_wo"][1])
    k.stage_peer(hs[4], out, I["norm_ffn"][1], I["peer_wq"][1], I["peer_skT"][1],
                 ub[1].rearrange("(i p) n -> i p n", p=128), vb[1], final_gamma=I["norm_final"])
    return k


def prep_shared(inputs):
    f = lambda a: np.ascontiguousarray(np.asarray(a, dtype=np.float32))
    P = inputs
    convwb = np.concatenate([
        np.concatenate([np.asarray(P['ssd_conv_w'])[0].T, np.asarray(P['ssd_conv_b'])[0][:, None]], 1),
        np.concatenate([np.asarray(P['ml_conv_w'])[0].T, np.asarray(P['ml_conv_b'])[0][:, None]], 1)], 0)
    sh = {
        "norm_mix": f(P['norm_mix']), "norm_xattn": f(P['norm_xattn']), "norm_mem": f(P['norm_mem']),
        "norm_ffn": f(P['norm_ffn']), "hgrn_lb_logits": f(P['hgrn_lb_logits']), "norm_final": f(P['norm_final']),
        "ev_w_in": f(np.asarray(P['ev_w_in'])[0]), "ev_w_out": f(np.asarray(P['ev_w_out'])[0]), "convwb": f(convwb),
        "ssd_dt_bias": f(np.asarray(P['ssd_dt_bias'])[0]), "ssd_a_log": f(np.asarray(P['ssd_a_log'])[0]),
        "ssd_d_skip": f(np.asarray(P['ssd_d_skip'])[0]), "ssd_norm": f(np.asarray(P['ssd_norm'])[0]),
        "ml_wq": f(np.asarray(P['ml_wq'])[0]), "ml_wk": f(np.asarray(P['ml_wk'])[0]), "ml_wv": f(np.asarray(P['ml_wv'])[0]),
        "ml_i_bias": f(np.asarray(P['ml_i_bias'])[0]), "ml_f_bias": f(np.asarray(P['ml_f_bias'])[0]),
        "ml_norm": f(np.asarray(P['ml_norm'])[0]),
        "od_w_in": f(np.asarray(P['od_w_in'])[0]), "od_w_out": f(np.asarray(P['od_w_out'])[0]),
        "hgrn_norm": f(np.asarray(P['hgrn_norm'])[0]),
        "xa_wq": f(P['xa_wq']), "xa_wkv": f(P['xa_wkv']), "xa_wo": f(P['xa_wo']),
        "peer_wq": f(P['peer_wq']),
        "peer_skT": f(np.asarray(P['peer_subkeys']).transpose(0, 4, 1, 2, 3).reshape(2, 128, 16, 128)),
        "peer_uR": f(np.asarray(P['peer_u']).reshape(2, 128, 128, 8, 128).transpose(0, 1, 4, 3, 2).reshape(2, 16384, 1024)),
        "peer_v": f(P['peer_v']),
    }
    return sh


def kernel(**inputs):
    x = np.asarray(inputs['x'], dtype=np.float32)
    mem = np.asarray(inputs['mem'], dtype=np.float32)
    B, T, _ = x.shape
    sh = prep_shared(inputs)
    k = build_full(T)
    in_maps = []
    for b in range(B):
        m = dict(sh)
        m["x"] = np.ascontiguousarray(x[b])
        m["mem"] = np.ascontiguousarray(mem[b])
        in_maps.append(m)
    res = run_bass_kernel_spmd(k.nc, in_maps, core_ids=list(range(B)))
    return np.stack([np.asarray(res.results[b]["out"], dtype=np.float32) for b in range(B)], 0)
```

```python
import numpy as np
from contextlib import ExitStack
import concourse.bass as bass
import concourse.mybir as mybir
from concourse.bass_utils import run_bass_kernel_spmd

F32 = mybir.dt.float32
BF16 = mybir.dt.bfloat16
U32 = mybir.dt.uint32
I32 = mybir.dt.int32
AF = mybir.ActivationFunctionType
ALU = mybir.AluOpType
AX = mybir.AxisListType

NDS = 24
WRITE_KEYS = ('out', 'accum_out', 'ap', 'out_max', 'out_indices')
EPS = 1e-6


class KB:
    def __init__(self):
        self.nc = bass.Bass("TRN2", target_bir_lowering=False)
        nc = self.nc
        self.engs = {'pe': nc.tensor, 'dve': nc.vector, 'act': nc.scalar, 'pool': nc.gpsimd, 'sp': nc.sync}
        self.sem = {k: nc.alloc_semaphore("s_" + k) for k in self.engs}
        self.cnt = {k: 0 for k in self.engs}
        self.seen = {k: {} for k in self.engs}
        self.dma_sems = [nc.alloc_semaphore("d%d" % i) for i in range(NDS)]
        self.dma_val = [0] * NDS
        self.dma_rr = 0
        self.bufs = {}
        self.gran = {}
        self.psum_names = set()
        self.rec = None
        self.self_skip = {}
        self.n_ins = 0
        self._uid = 0
        self.stack = None
        self._scope = None
        self.profile_scopes = False

    def sb(self, shape, dt=F32, name=None, gran=None):
        self._uid += 1
        nm = "%s_%d" % (name or "sb", self._uid)
        if gran is not None:
            self.gran[nm] = gran * (2 if dt == BF16 else 4)
        if self.stack is not None:
            return self.stack.enter_context(self.nc.sbuf_tensor(nm, list(shape), dt))
        return self.nc.alloc_sbuf_tensor(nm, list(shape), dt)

    def ps(self, shape, dt=F32, name=None, gran=None):
        self._uid += 1
        nm = "%s_%d" % (name or "ps", self._uid)
        self.gran[nm] = 2048
        self.psum_names.add(nm)
        if self.stack is not None:
            return self.stack.enter_context(self.nc.psum_tensor(nm, list(shape), dt))
        return self.nc.alloc_psum_tensor(nm, list(shape), dt)

    def ring(self, n, shape, dt=F32, name=None, psum=False, gran=None):
        return [(self.ps if psum else self.sb)(shape, dt, name, gran=gran) for _ in range(n)]

    def begin_stage(self):
        self.barrier()
        self.stack = ExitStack()

    def end_stage(self):
        self.barrier()
        self.stack.close()
        self.stack = None

    def scope(self, name):
        if getattr(self, '_scope', None) is not None:
            self.nc.leave_named_scope(self._scope_tok) if False else self._scope.__exit__(None, None, None)
            self._scope = None
        if name is not None and self.profile_scopes:
            self._scope = self.nc.named_scope(name)
            self._scope.__enter__()

    def dram(self, name, shape, dt=F32, kind="Internal"):
        return self.nc.dram_tensor(name, list(shape), dt, kind=kind)

    def semh(self, sk):
        return self.sem[sk[1]] if sk[0] == 'e' else self.dma_sems[sk[1]]

    def slots(self, ap):
        t = ap.tensor
        n = t.name
        g = self.gran.get(n)
        if g is None:
            return [n]
        apl = list(ap.ap)
        F_ = int(apl[0][0])
        isz = 2 if ap.dtype == BF16 else 4
        off = int(ap.offset) % F_ if F_ > 0 else int(ap.offset)
        lo = hi = off
        for (st, num) in apl[1:]:
            st = int(st); num = int(num)
            if st >= 0:
                hi += st * (num - 1)
            else:
                lo += st * (num - 1)
        return [(n, j) for j in range(lo * isz // g, hi * isz // g + 1)]

    def bufl(self, ap):
        out = []
        for key in self.slots(ap):
            b = self.bufs.get(key)
            if b is None:
                b = {'w': None, 'r': {}}
                self.bufs[key] = b
            out.append(b)
        return out

    def emit(self, eng, fn, reads, writes, is_dma=False):
        deps = []
        rb = [b for t in reads for b in self.bufl(t)]
        wb = [b for t in writes for b in self.bufl(t)]
        for t in reads:
            if t.tensor.name in self.psum_names:
                for b in self.bufl(t):
                    deps.extend((sk_, v_) for sk_, v_ in b['r'].items() if sk_ != ('e', eng))
        for b in rb:
            if b['w']:
                deps.append(b['w'])
        for b in wb:
            if b['w']:
                deps.append(b['w'])
            deps.extend(b['r'].items())
        E = self.engs[eng]
        seen = self.seen[eng]
        for (sk, val) in deps:
            if eng == 'pe' and sk == ('e', 'pe'):
                continue
            if seen.get(sk, 0) >= val:
                continue
            if sk == ('e', eng) and eng in self.self_skip and self.cnt[eng] - val >= self.self_skip[eng]:
                continue
            E.wait_ge(self.semh(sk), val)
            seen[sk] = val
        if is_dma:
            i = self.dma_rr
            self.dma_rr = (i + 1) % NDS
            sk = ('d', i)
            pv = self.dma_val[i]
            if pv and seen.get(sk, 0) < pv:
                E.wait_ge(self.dma_sems[i], pv)
                seen[sk] = pv
            ins = fn(E)
            ins.then_inc(self.dma_sems[i], 16)
            self.dma_val[i] += 16
            tok = (sk, self.dma_val[i])
        else:
            ins = fn(E)
            ins.then_inc(self.sem[eng], 1)
            self.cnt[eng] += 1
            tok = (('e', eng), self.cnt[eng])
        self.n_ins += 1
        for b in rb:
            r = b['r']
            if r.get(tok[0], 0) < tok[1]:
                r[tok[0]] = tok[1]
        for b in wb:
            b['w'] = tok
            b['r'] = {}
        return ins

    def op(self, eng, name, **kw):
        if self.rec is not None:
            c = 0
            if eng == 'dve':
                o = kw.get('out', kw.get('ap'))
                n = 1
                if o is not None:
                    for d_ in list(o.shape)[1:]:
                        n *= int(d_)
                c = 70 + (2 * n if name == 'tensor_tensor' else n)
            self.rec.append((eng, name, kw, c))
            return None
        reads = [v for k_, v in kw.items() if k_ not in WRITE_KEYS and hasattr(v, 'tensor')]
        writes = [v for k_, v in kw.items() if k_ in WRITE_KEYS and v is not None and hasattr(v, 'tensor')]
        return self.emit(eng, lambda E: getattr(E, name)(**kw), reads, writes, is_dma=(name == 'dma_start'))

    def replay(self, ops, n=None):
        assert self.rec is None
        if n is None:
            n = len(ops)
        for (eng, name, kw, c) in ops[:n]:
            if eng != 'marker':
                self.op(eng, name, **kw)
        return ops[n:]

    def replay_cost(self, ops, budget, chunk=None):
        acc = 0
        j = 0
        while j < len(ops) and acc < budget:
            eng, name, kw, c = ops[j]
            if eng == 'marker':
                if chunk is not None and chunk < name:
                    break
                j += 1
                continue
            self.op(eng, name, **kw)
            acc += c
            j += 1
        return ops[j:]

    def dma(self, out, in_, eng='sp', **kw):
        return self.op(eng, 'dma_start', out=out, in_=in_, **kw)

    def barrier(self):
        for eng, E in self.engs.items():
            seen = self.seen[eng]
            for e2 in self.engs:
                sk = ('e', e2)
                v = self.cnt[e2]
                if e2 == eng and eng == 'pe':
                    continue
                if v and seen.get(sk, 0) < v:
                    E.wait_ge(self.sem[e2], v)
                    seen[sk] = v
            for i in range(NDS):
                sk = ('d', i)
                v = self.dma_val[i]
                if v and seen.get(sk, 0) < v:
                    E.wait_ge(self.dma_sems[i], v)
                    seen[sk] = v
        for b in self.bufs.values():
            b['w'] = None
            b['r'] = {}

    def mm(self, out, lhsT, rhs, start=True, stop=True):
        return self.op('pe', 'matmul', out=out, lhsT=lhsT, rhs=rhs, start=start, stop=stop)

    def tr(self, out, in_, ident):
        return self.op('pe', 'transpose', out=out, in_=in_, identity=ident)

    def act(self, out, in_, func, bias=None, scale=None, accum_out=None, eng='act'):
        kw = dict(out=out, in_=in_, func=func)
        if bias is not None:
            kw['bias'] = bias
        if scale is not None:
            kw['scale'] = scale
        if accum_out is not None:
            kw['accum_out'] = accum_out
        return self.op('act', 'activation', **kw)

    def tt(self, out, in0, in1, op, eng='dve'):
        return self.op(eng, 'tensor_tensor', out=out, in0=in0, in1=in1, op=op)

    def ts(self, out, in0, s1, op0, s2=None, op1=None, eng='dve', accum_out=None):
        kw = dict(out=out, in0=in0, scalar1=s1, scalar2=s2, op0=op0)
        if op1 is not None:
            kw['op1'] = op1
        if accum_out is not None:
            kw['accum_out'] = accum_out
        return self.op(eng, 'tensor_scalar', **kw)

    def stt(self, out, in0, scalar, in1, op0, op1):
        return self.op('dve', 'scalar_tensor_tensor', out=out, in0=in0, scalar=scalar, in1=in1, op0=op0, op1=op1)

    def copy(self, out, in_, eng='dve'):
        if eng == 'act':
            return self.op('act', 'activation', out=out, in_=in_, func=AF.Copy)
        return self.op(eng, 'tensor_copy', out=out, in_=in_)

    def memset(self, ap, c, eng='dve'):
        return self.op(eng, 'memset', ap=ap, constant=c)


D = 1024
E_IN = 4632


class MK(KB):
    def __init__(self, T, dbg=()):
        super().__init__()
        self.T = T
        self.NT = T // 128
        self.dbg = set(dbg)
        self.inputs = {}
        self.scr = {}
        self.x_pool_every = 10 ** 9; self.x_pm_eng = 'dve'; self.x_v_eng = 'act'; self.x_st_eng = 'pool'

    def inp(self, name, shape, dt=F32):
        t = self.dram(name, shape, dt, kind="ExternalInput")
        self.inputs[name] = t
        return t

    def scratch(self, name, shape, dt=F32):
        t = self.dram(name, shape, dt, kind=("ExternalOutput" if name in self.dbg else "Internal"))
        self.scr[name] = t
        return t

    def make_consts(self):
        k = self
        self.ones = k.sb([128, 128], F32, "ones")
        k.memset(self.ones[:], 1.0)
        self.idf = k.sb([128, 128], F32, "idf")
        k.op('pool', 'affine_select', out=self.idf[:], in_=self.ones[:], pattern=[[-1, 128]],
             compare_op=ALU.is_equal, fill=0.0, base=0, channel_multiplier=1)
        self.ident = k.sb([128, 128], BF16, "ident")
        k.copy(self.ident[:], self.idf[:])
        self.triu = k.sb([128, 128], F32, "triu")
        k.op('pool', 'affine_select', out=self.triu[:], in_=self.ones[:], pattern=[[1, 128]],
             compare_op=ALU.is_ge, fill=0.0, base=0, channel_multiplier=-1)
        self.ntriu = k.sb([128, 128], F32, "ntriu")
        k.ts(self.ntriu[:], self.triu[:], -1.0, ALU.mult)
        self.mhalf = k.sb([128, 16], F32, "mhalf")
        k.memset(self.mhalf[:], -0.5)
        self.zeros = k.sb([128, 512], F32, "zeros")
        k.memset(self.zeros[:], 0.0)

    def bcast_row(self, dst, src_ap_1d):
        self.dma(dst, src_ap_1d.partition_broadcast(128))

    def rms_T(self, xt, gb, hn, hT, tp, ss, sq):
        k = self
        k.act(sq[:], xt[:], AF.Square, accum_out=ss[:, 0:1])
        k.ts(ss[:, 1:2], ss[:, 0:1], 1.0 / D, ALU.mult, EPS, ALU.add)
        k.tt(ss[:, 3:4], ss[:, 1:2], self.mhalf[:, 0:1], ALU.pow, eng='pool')
        k.stt(hn[:], xt[:], ss[:, 3:4], gb[:], ALU.mult, ALU.mult)
        for c in range(8):
            k.tr(tp[:, c, :], hn[:, c * 128:(c + 1) * 128], self.ident[:])
        k.copy(hT, tp[:], eng='act')

    def stage_inproj_even(self, h_src, w_in, gamma, side_fn=None):
        k = self
        T, NT = self.T, self.NT
        z_s = k.scratch("z_s", [T, 1024])
        cT_s = k.scratch("cT_s", [2560, T + 3])
        dt_s = k.scratch("dt_s", [T, 16])
        og_s = k.scratch("og_s", [T, 1024])
        gT_s = k.scratch("gT_s", [8, T])
        k.begin_stage()
        W = k.sb([128, 8, E_IN], BF16, "Win")
        for c in range(8):
            k.dma(W[:, c, :], w_in[c * 128:(c + 1) * 128, :], eng='pool')
        gb = k.sb([128, D], F32, "gb")
        k.bcast_row(gb[:], gamma)
        for c in range(20):
            k.dma(cT_s.ap()[c * 128:(c + 1) * 128, 0:3], self.zeros[:, 0:3])
        xts = k.ring(2, [128, D], F32, "xt")
        sqs = k.ring(1, [128, D], F32, "sq")
        sss = k.ring(2, [128, 4], F32, "ss")
        hns = k.ring(2, [128, D], BF16, "hn")
        hTs = k.ring(2, [128, 8, 128], BF16, "hT")
        tps = k.ring(1, [128, 8, 128], BF16, "tp", psum=True)
        pss = k.ring(4, [128, 512], F32, "pp", psum=True)
        psf = k.ring(2, [128, 4, 128], F32, "pf", psum=True)
        otok = k.ring(2, [128, 1024 + 16 + 1024], F32, "otok")
        ofm = k.ring(2, [128, 20, 128], F32, "ofm")
        og8 = k.ring(2, [8, 128], F32, "og8")
        groups = [(0, 512, 0), (512, 512, 512), (2560, 16, 1024), (3600, 512, 1040), (4112, 512, 1552)]
        nps = 0
        side = side_fn() if side_fn is not None else []
        per = (len(side) + NT - 1) // NT
        for i in range(NT):
            side = k.replay(side, per)
            xt = xts[i % 2]; hn = hns[i % 2]; hT = hTs[i % 2]; ss = sss[i % 2]
            k.dma(xt[:], h_src[i * 128:(i + 1) * 128, :])
            k.rms_T(xt, gb, hn, hT[:], tps[0], ss, sqs[0])
            ot = otok[i % 2]
            for (c0, n, d0) in groups:
                p = pss[nps % 4]; nps += 1
                for c in range(8):
                    k.mm(p[:, 0:n], hT[:, c, :], W[:, c, c0:c0 + n], start=(c == 0), stop=(c == 7))
                k.copy(ot[:, d0:d0 + n], p[:, 0:n], eng=('act' if nps % 2 else 'dve'))
            k.dma(z_s.ap()[i * 128:(i + 1) * 128, :], ot[:, 0:1024], eng='pool')
            k.dma(dt_s.ap()[i * 128:(i + 1) * 128, :], ot[:, 1024:1040], eng='pool')
            k.dma(og_s.ap()[i * 128:(i + 1) * 128, :], ot[:, 1040:2064], eng='pool')
            of = ofm[i % 2]
            for ch in range(20):
                f0 = 1024 + ch * 128 if ch < 12 else 2576 + (ch - 12) * 128
                p = psf[(ch // 4) % 2]
                for c in range(8):
                    k.mm(p[:, ch % 4, :], W[:, c, f0:f0 + 128], hT[:, c, :], start=(c == 0), stop=(c == 7))
                if ch % 4 == 3:
                    k.copy(of[:, ch - 3:ch + 1, :], p[:], eng=('act' if (ch // 4) % 2 else 'dve'))
            k.dma(cT_s.ap()[:, 3 + i * 128:3 + (i + 1) * 128].rearrange("(ch p) t -> p ch t", p=128), of[:], eng='pool')
            p = pss[nps % 4]; nps += 1
            for c in range(8):
                k.mm(p[0:8, 0:128], W[:, c, 4624:4632], hT[:, c, :], start=(c == 0), stop=(c == 7))
            k.copy(og8[i % 2][:], p[0:8, 0:128])
            k.dma(gT_s.ap()[:, i * 128:(i + 1) * 128], og8[i % 2][:], eng='pool')
        k.replay(side)
        k.end_stage()

    def stage_ssd(self, convwb, dt_bias, a_log, d_skip, ssd_norm, own=True):
        k = self
        T, NT = self.T, self.NT
        z_s, cT_s, dt_s = k.scr["z_s"], k.scr["cT_s"], k.scr["dt_s"]
        ya_s = k.scr["ya_s"] if "ya_s" in k.scr else k.scratch("ya_s", [T, 2048], BF16)
        if own:
            k.begin_stage()
        cwb = k.sb([128, 12, 5], F32, "cwb")
        k.dma(cwb[:], convwb[0:1536, :].rearrange("(ch p) k -> p ch k", p=128))
        dtb = k.sb([128, 16], F32, "dtb"); k.bcast_row(dtb[:], dt_bias)
        a_b = k.sb([128, 16], F32, "a_b"); k.bcast_row(a_b[:], a_log)
        k.act(a_b[:], a_b[:], AF.Exp)
        k.ts(a_b[:], a_b[:], -1.0, ALU.mult)
        dsk = k.sb([128, 16], F32, "dsk"); k.bcast_row(dsk[:], d_skip)
        nrm = k.sb([128, 1024], F32, "nrm"); k.bcast_row(nrm[:], ssd_norm)
        S = k.sb([128, 1024], F32, "S"); k.memset(S[:], 0.0)
        Sb = k.sb([128, 1024], BF16, "Sb"); k.memset(Sb[:], 0.0)
        xins = k.ring(2, [128, 12, 131], F32, "xin")
        acc = k.sb([128, 12, 128], F32, "acc", gran=128)
        xcvs = k.ring(2, [128, 12, 128], BF16, "xcv")
        xs_tok = k.sb([128, 1024], BF16, "xs_tok")
        bm_tok = k.sb([128, 256], BF16, "bm_tok")
        sm = k.ring(2, [128, 8, 16], F32, "sm")
        dAb = k.sb([128, 16, 128], F32, "dAb")
        cst = k.sb([128, 32], F32, "cst")
        ex = k.sb([128, 3, 16], F32, "ex")
        xdt = k.sb([128, 1024], BF16, "xdt")
        xsk = k.sb([128, 1024], F32, "xsk")
        xw = k.sb([128, 1024], BF16, "xw")
        maskcb = k.sb([128, 2, 128], F32, "maskcb")
        Dms = k.ring(2, [128, 4, 128], F32, "Dm")
        Es = k.ring(2, [128, 4, 128], F32, "E")
        Ms = k.ring(2, [128, 4, 128], BF16, "M")
        y1 = k.sb([128, 1024], F32, "y1")
        zts = k.ring(2, [128, 1024], F32, "zt")
        sz = k.sb([128, 1024], F32, "sz")
        junk = k.sb([128, 512], F32, "junk")
        ssq = k.sb([128, 8], F32, "ssq")
        yas = k.ring(2, [128, 1024], BF16, "ya")
        csb = k.ps([128, 512], F32, "csb")
        tp = csb[:].bitcast(BF16).rearrange("p (a b) -> p a b", a=8)
        Dps = k.ring(1, [128, 4, 128], F32, "Dp", psum=True)
        Y_ps = k.ps([128, 512], F32, "Yps")
        YC = k.ps([128, 512], F32, "YC")

        def b64(ap16):
            return ap16.unsqueeze(2).to_broadcast([128, 16, 64])

        def v64(ap1024):
            return ap1024.rearrange("p (h d) -> p h d", d=64)

        def tile(i):
            xin = xins[i % 2]; xcv = xcvs[i % 2]; s_ = sm[i % 2]
            k.dma(xin[:], cT_s.ap()[0:1536, i * 128:i * 128 + 131].rearrange("(ch p) t -> p ch t", p=128))
            dtr = s_[:, 0, :]
            k.dma(dtr, dt_s.ap()[i * 128:(i + 1) * 128, :])
            zt = zts[i % 2]
            k.dma(zt[:], z_s.ap()[i * 128:(i + 1) * 128, :])
            for ch in range(12):
                k.ts(acc[:, ch, :], xin[:, ch, 3:131], cwb[:, ch, 3:4], ALU.mult, cwb[:, ch, 4:5], ALU.add)
            for kk in range(3):
                for ch in range(12):
                    k.stt(acc[:, ch, :], xin[:, ch, kk:kk + 128], cwb[:, ch, kk:kk + 1], acc[:, ch, :], ALU.mult, ALU.add)
            k.act(xcv[:], acc[:], AF.Silu)
            for c in range(8):
                k.tr(tp[:, c, :], xcv[:, c, :], self.ident[:])
            k.copy(xs_tok[:], tp.rearrange("p c t -> p (c t)"), eng='act')
            for g in range(2):
                k.tr(tp[:, g, :], xcv[:, 8 + g, :], self.ident[:])
            k.copy(bm_tok[:], tp[:, 0:2, :].rearrange("p c t -> p (c t)"), eng='act')
            t1 = s_[:, 1, :]; ab = s_[:, 2, :]; e_ = s_[:, 3, :]; l_ = s_[:, 4, :]; dt = s_[:, 5, :]; dA = s_[:, 6, :]
            k.tt(t1, dtr, dtb[:], ALU.add)
            k.act(ab, t1, AF.Abs)
            k.act(e_, ab, AF.Exp, scale=-1.0)
            k.act(l_, e_, AF.Ln, bias=1.0)
            k.stt(dt, t1, 0.0, l_, ALU.max, ALU.add)
            k.tt(dA, dt, a_b[:], ALU.mult)
            k.copy(dAb[:], dA.unsqueeze(2).to_broadcast([128, 16, 128]), eng='pool')
            k.mm(csb[:, 0:16], self.triu[:], dA)
            k.mm(csb[:, 16:32], self.ones[:], dA)
            k.copy(cst[:], csb[:, 0:32])
            k.act(ex[:, 0, :], cst[:, 0:16], AF.Exp)
            k.act(ex[:, 1, :], cst[:, 16:32], AF.Exp)
            k.tt(s_[:, 7, :], cst[:, 16:32], cst[:, 0:16], ALU.subtract)
            k.act(ex[:, 2, :], s_[:, 7, :], AF.Exp)
            k.tt(v64(xdt[:]), v64(xs_tok[:]), b64(dt), ALU.mult)
            k.tt(v64(xsk[:]), v64(xs_tok[:]), b64(dsk[:]), ALU.mult, eng='pool')
            k.tt(v64(xw[:]), v64(xdt[:]), b64(ex[:, 2, :]), ALU.mult, eng='pool')
            for g in range(2):
                k.mm(csb[:, 128 + g * 128:256 + g * 128], xcv[:, 8 + g, :], xcv[:, 10 + g, :])
            k.tt(maskcb[:], csb[:, 128:384].rearrange("p (g l) -> p g l", g=2),
                 self.triu[:].unsqueeze(1).to_broadcast([128, 2, 128]), ALU.mult)
            def h8(ap512):
                return ap512.rearrange("p (h d) -> p h d", d=64)

            for g in range(2):
                gs = slice(g * 512, (g + 1) * 512)
                for hg in (2 * g, 2 * g + 1):
                    Dp = Dps[0]; Dm = Dms[hg % 2]; E = Es[hg % 2]; M = Ms[hg % 2]
                    for hh in range(4):
                        h = hg * 4 + hh
                        k.mm(Dp[:, hh, :], dAb[:, h, :], self.triu[:], start=True, stop=False)
                        k.mm(Dp[:, hh, :], self.ntriu[:], dAb[:, h, :], start=False, stop=True)
                    k.ts(Dm[:], Dp[:], 0.0, ALU.min)
                    k.act(E[:], Dm[:], AF.Exp)
                    k.tt(M[:], E[:], maskcb[:, g, :].unsqueeze(1).to_broadcast([128, 4, 128]), ALU.mult, eng='pool')
                    for hh in range(4):
                        h = hg * 4 + hh
                        k.mm(Y_ps[:, (h % 8) * 64:(h % 8 + 1) * 64], M[:, hh, :], xdt[:, h * 64:(h + 1) * 64])
                k.mm(YC[:], xcv[:, 10 + g, :], Sb[:, gs])
                k.tt(h8(y1[:, gs]), h8(YC[:]), ex[:, 0, g * 8:(g + 1) * 8].unsqueeze(2).to_broadcast([128, 8, 64]), ALU.mult)
                k.tt(y1[:, gs], y1[:, gs], Y_ps[:], ALU.add)
                k.mm(YC[:], bm_tok[:, g * 128:(g + 1) * 128], xw[:, gs])
                k.tt(h8(S[:, gs]), h8(S[:, gs]), ex[:, 1, g * 8:(g + 1) * 8].unsqueeze(2).to_broadcast([128, 8, 64]), ALU.mult)
                k.tt(S[:, gs], S[:, gs], YC[:], ALU.add)
                k.copy(Sb[:, gs], S[:, gs], eng='act')
            k.tt(y1[:], y1[:], xsk[:], ALU.add)
            k.act(sz[:], zt[:], AF.Silu)
            k.tt(y1[:], y1[:], sz[:], ALU.mult)
            for g in range(2):
                k.act(junk[:], y1[:, g * 512:(g + 1) * 512], AF.Square, accum_out=ssq[:, g:g + 1])
            k.ts(ssq[:, 2:4], ssq[:, 0:2], 1.0 / 512, ALU.mult, EPS, ALU.add)
            k.tt(ssq[:, 6:8], ssq[:, 2:4], self.mhalf[:, 0:2], ALU.pow, eng='pool')
            ya = yas[i % 2]
            for g in range(2):
                k.stt(ya[:, g * 512:(g + 1) * 512], y1[:, g * 512:(g + 1) * 512], ssq[:, 6 + g:7 + g],
                      nrm[:, g * 512:(g + 1) * 512], ALU.mult, ALU.mult)
            k.dma(ya_s.ap()[i * 128:(i + 1) * 128, 0:1024], ya[:], eng='pool')

        if not own:
            return tile
        for i in range(NT):
            tile(i)
        k.end_stage()

    def stage_mlstm(self, convwb, wq, wk, wv, i_bias, f_bias, ml_norm, own=True):
        k = self
        T, NT = self.T, self.NT
        NC = NT; HC = 4 * NC
        cT_s, gT_s, og_s, ya_s = k.scr["cT_s"], k.scr["gT_s"], k.scr["og_s"], k.scr["ya_s"]
        idf, ones, triu = self.idf, self.ones, self.triu
        if own:
            k.begin_stage()
        cwb = k.sb([128, 8, 5], F32, "cwb")
        k.dma(cwb[:], convwb[1536:2560, :].rearrange("(ch p) k -> p ch k", p=128))
        Ws = []
        for nm, w in (("Wq", wq), ("Wk", wk), ("Wv", wv)):
            Wt_ = k.sb([128, 8, 256], BF16, nm)
            k.dma(Wt_[:], w.rearrange("h (dc p) e -> p (h dc) e", p=128), eng='pool')
            Ws.append(Wt_)
        Wq, Wk, Wv = Ws
        nrm = k.sb([128, 1024], F32, "nrm"); k.bcast_row(nrm[:], ml_norm)
        proj = k.ps([128, 1024], F32, "proj")
        p8 = proj[:].rearrange("p (a b) -> p a b", a=8)
        DQ = k.ps([128, 512], F32, "DQ")
        intra = k.ps([128, 512], F32, "intra")
        Dp = DQ
        clp = proj[:].rearrange("p (a b) -> p a b", a=2)
        G = k.sb([HC, 2, 128], F32, "G")
        k.dma(G[:, 0, :], gT_s.ap()[0:4, :].rearrange("h (c l) -> (h c) l", l=128))
        k.dma(G[:, 1, :], gT_s.ap()[4:8, :].rearrange("h (c l) -> (h c) l", l=128))
        bc = k.sb([HC, 2], F32, "bc")
        for h in range(4):
            k.dma(bc[h * NC:(h + 1) * NC, 0:1], i_bias[h:h + 1].partition_broadcast(NC))
            k.dma(bc[h * NC:(h + 1) * NC, 1:2], f_bias[h:h + 1].partition_broadcast(NC))
        pp = k.sb([HC, 12, 128], F32, "prepass")
        xf, ab, e_, l1, logf, it, bcum, u, cmx, mintra, gend, wend = [pp[:, j, :] for j in range(12)]
        pq2 = k.sb([HC, 6, 128], F32, "prepass2")
        mt, t3, at, emt, rowt, _sp = [pq2[:, j, :] for j in range(6)]
        k.ts(xf, G[:, 1, :], bc[:, 1:2], ALU.add)
        k.act(ab, xf, AF.Abs)
        k.act(e_, ab, AF.Exp, scale=-1.0)
        k.act(l1, e_, AF.Ln, bias=1.0)
        k.stt(logf, xf, 0.0, l1, ALU.min, ALU.subtract)
        k.ts(it, G[:, 0, :], bc[:, 0:1], ALU.add)
        k.op('dve', 'tensor_tensor_scan', out=bcum, data0=ones[:HC, :], data1=logf, initial=0.0, op0=ALU.mult, op1=ALU.add)
        k.tt(u, it, bcum, ALU.subtract)
        k.op('dve', 'tensor_tensor_scan', out=cmx, data0=u, data1=u, initial=-1e30, op0=ALU.max, op1=ALU.max)
        k.tt(mintra, bcum, cmx, ALU.add)
        k.ts(gend, u, pp[:, 6, 127:128], ALU.add)
        sc = k.sb([HC, 4], F32, "sc")
        k.op('dve', 'tensor_reduce', out=sc[:, 0:1], in_=gend, axis=AX.X, op=ALU.max)
        k.ts(sc[:, 1:2], sc[:, 0:1], -1.0, ALU.mult)
        k.act(wend, gend, AF.Exp, bias=sc[:, 1:2])
        rp = DQ
        k.tr(rp[0:1, 0:HC], pp[:, 6, 127:128], idf[:HC, :HC])
        k.tr(rp[0:1, 128:128 + HC], sc[:, 0:1], idf[:HC, :HC])
        rows = k.sb([1, 8, 128], F32, "rows")
        k.memset(rows[:], 0.0)
        k.copy(rows[0:1, 0, 0:HC], rp[0:1, 0:HC])
        k.copy(rows[0:1, 1, 0:HC], rp[0:1, 128:128 + HC])
        for h in range(4):
            seg = slice(h * NC, (h + 1) * NC)
            k.op('dve', 'tensor_tensor_scan', out=rows[0:1, 2, seg], data0=rows[0:1, 0, seg], data1=rows[0:1, 1, seg],
                 initial=0.0, op0=ALU.add, op1=ALU.max)
        if NC > 1:
            for h in range(4):
                k.copy(rows[0:1, 3, h * NC + 1:(h + 1) * NC], rows[0:1, 2, h * NC:(h + 1) * NC - 1])
        k.tt(rows[0:1, 4, :], rows[0:1, 0, :], rows[0:1, 3, :], ALU.add)
        k.tt(rows[0:1, 4, :], rows[0:1, 4, :], rows[0:1, 2, :], ALU.subtract)
        k.act(rows[0:1, 5, :], rows[0:1, 4, :], AF.Exp)
        k.tt(rows[0:1, 7, :], rows[0:1, 1, :], rows[0:1, 2, :], ALU.subtract)
        k.act(rows[0:1, 6, :], rows[0:1, 7, :], AF.Exp)
        ABp = intra
        k.mm(ABp[:, 0:256], ones[0:1, :], rows[0:1, 5:7, :].rearrange("p a b -> p (a b)"))
        AB = k.sb([128, 2, 128], F32, "AB")
        k.copy(AB[:], ABp[:, 0:256].rearrange("p (a b) -> p a b", a=2))
        k.mm(rp[0:HC, 256:257], rows[0:1, 3, 0:HC], ones[0:1, 0:1])
        k.copy(sc[:, 2:3], rp[0:HC, 256:257])
        mpc = sc[:, 2:3]
        k.stt(mt, bcum, mpc, mintra, ALU.add, ALU.max)
        k.stt(t3, bcum, mpc, mt, ALU.add, ALU.subtract)
        k.act(at, t3, AF.Exp)
        k.act(emt, mt, AF.Exp, scale=-1.0)
        k.tt(rowt, bcum, mt, ALU.subtract)
        cp2 = proj
        k.tr(cp2[:, 0:HC], at, idf[:HC, :HC])
        k.tr(cp2[:, 128:128 + HC], emt, idf[:HC, :HC])
        k.tr(cp2[:, 256:256 + HC], wend, idf[:HC, :HC])
        cols = k.sb([128, 3, 128], F32, "cols")
        k.memset(cols[:], 0.0)
        k.copy(cols[:, :, 0:HC], cp2[:, 0:384].rearrange("p (a b) -> p a b", a=3)[:, :, 0:HC])
        if own:
            k.barrier()
        C = k.sb([128, 8, 257], F32, "C"); k.memset(C[:], 0.0)
        Cb = k.sb([128, 8, 257], BF16, "Cb"); k.memset(Cb[:], 0.0)
        vext = k.sb([128, 4, 257], BF16, "vext"); k.memset(vext[:], 1.0)
        xins = k.ring(2, [128, 8, 131], F32, "xin")
        acc = k.sb([128, 8, 128], F32, "acc", gran=128)
        xc = k.sb([128, 8, 128], BF16, "xc")
        xmr = k.sb([128, 8, 128], BF16, "xmr")
        ogs = k.ring(2, [128, 1024], F32, "og")
        sg = k.sb([128, 1024], F32, "sg")
        qT = k.sb([128, 8, 128], BF16, "qT")
        kT = k.sb([128, 8, 128], BF16, "kT")
        kw = k.sb([128, 4, 256], BF16, "kw")
        Dms = k.ring(2, [128, 128], F32, "Dm")
        Es = k.ring(2, [128, 128], F32, "E")
        Ems = k.ring(2, [128, 128], F32, "Em")
        Wts = k.ring(2, [128, 128], BF16, "Wt")
        isb = k.sb([128, 257], F32, "isb")
        num = k.sb([128, 257], F32, "num")
        s8 = k.sb([128, 8], F32, "s8")
        junk = k.sb([128, 256], F32, "junk")
        hn_ = k.sb([128, 256], F32, "hn_")
        ybs = k.ring(2, [128, 1024], BF16, "yb")
        def tile(c):
            xin = xins[c % 2]; og = ogs[c % 2]; yb = ybs[c % 2]
            k.dma(xin[:], cT_s.ap()[1536:2560, c * 128:c * 128 + 131].rearrange("(ch p) t -> p ch t", p=128))
            k.dma(og[:], og_s.ap()[c * 128:(c + 1) * 128, :])
            for ch in range(8):
                k.ts(acc[:, ch, :], xin[:, ch, 3:131], cwb[:, ch, 3:4], ALU.mult, cwb[:, ch, 4:5], ALU.add)
            for kk in range(3):
                for ch in range(8):
                    k.stt(acc[:, ch, :], xin[:, ch, kk:kk + 128], cwb[:, ch, kk:kk + 1], acc[:, ch, :], ALU.mult, ALU.add)
            k.act(xc[:], acc[:], AF.Silu)
            k.copy(xmr[:], xin[:, :, 3:131], eng='pool')
            k.act(sg[:], og[:], AF.Sigmoid)
            for (Wm, dst, scl) in ((Wq, qT, None), (Wk, kT, 1.0 / 16)):
                for h in range(4):
                    for ec in range(2):
                        for dc in range(2):
                            k.mm(p8[:, h * 2 + ec, :], Wm[:, h * 2 + dc, ec * 128:(ec + 1) * 128], xc[:, h * 2 + dc, :],
                                 start=(dc == 0), stop=(dc == 1))
                if scl is None:
                    k.copy(dst[:], p8, eng='act')
                else:
                    k.ts(dst[:], p8, scl, ALU.mult)
            for h in range(4):
                for dc in range(2):
                    k.mm(proj[:, h * 256:(h + 1) * 256], xmr[:, h * 2 + dc, :], Wv[:, h * 2 + dc, :], start=(dc == 0), stop=(dc == 1))
            k.copy(vext[:, :, 0:256], proj[:].rearrange("p (h e) -> p h e", h=4), eng='act')
            for h in range(4):
                for dc in range(2):
                    k.mm(proj[:, h * 256:(h + 1) * 256], xc[:, h * 2 + dc, :], Wk[:, h * 2 + dc, :], start=(dc == 0), stop=(dc == 1))
            for h in range(4):
                hc = h * NC + c
                k.ts(kw[:, h, :], proj[:, h * 256:(h + 1) * 256], cols[:, 2, hc:hc + 1], ALU.mult, 1.0 / 16, ALU.mult)
            for h in range(4):
                hc = h * NC + c
                Dm = Dms[h % 2]; E = Es[h % 2]; Em = Ems[h % 2]; Wt = Wts[h % 2]
                sel = idf[:HC, hc:hc + 1].to_broadcast([HC, 128])
                k.mm(Dp[:, 0:128], sel, rowt, start=True, stop=False)
                k.mm(Dp[:, 0:128], u, sel, start=False, stop=True)
                k.ts(Dm[:], Dp[:, 0:128], 0.0, ALU.min)
                k.act(E[:], Dm[:], AF.Exp)
                for ec in range(2):
                    k.mm(DQ[:, 128:256], kT[:, h * 2 + ec, :], qT[:, h * 2 + ec, :], start=(ec == 0), stop=(ec == 1))
                k.tt(Em[:], E[:], triu[:], ALU.mult, eng='pool')
                k.tt(Wt[:], Em[:], DQ[:, 128:256], ALU.mult)
                for dc in range(2):
                    k.mm(DQ[:, 256:512], qT[:, h * 2 + dc, :], Cb[:, h * 2 + dc, 0:256], start=(dc == 0), stop=(dc == 1))
                k.mm(intra[:, 0:257], Wt[:], vext[:, h, :])
                for dc in range(2):
                    k.mm(intra[:, 384:385], qT[:, h * 2 + dc, :], Cb[:, h * 2 + dc, 256:257], start=(dc == 0), stop=(dc == 1))
                k.copy(isb[:], intra[:, 0:257], eng='act')
                k.stt(num[:, 0:256], DQ[:, 256:512], cols[:, 0, hc:hc + 1], isb[:, 0:256], ALU.mult, ALU.add)
                k.stt(num[:, 256:257], intra[:, 384:385], cols[:, 0, hc:hc + 1], isb[:, 256:257], ALU.mult, ALU.add)
                k.act(s8[:, 0:1], num[:, 256:257], AF.Abs)
                k.tt(s8[:, 1:2], s8[:, 0:1], cols[:, 1, hc:hc + 1], ALU.max)
                k.op('dve', 'reciprocal', out=s8[:, 2:3], in_=s8[:, 1:2])
                k.act(junk[:], num[:, 0:256], AF.Square, scale=s8[:, 2:3], accum_out=s8[:, 3:4])
                k.ts(s8[:, 4:5], s8[:, 3:4], 1.0 / 256, ALU.mult, EPS, ALU.add)
                k.tt(s8[:, 6:7], s8[:, 4:5], self.mhalf[:, 0:1], ALU.pow, eng='pool')
                k.tt(s8[:, 7:8], s8[:, 6:7], s8[:, 2:3], ALU.mult)
                k.stt(hn_[:], num[:, 0:256], s8[:, 7:8], nrm[:, h * 256:(h + 1) * 256], ALU.mult, ALU.mult)
                k.tt(yb[:, h * 256:(h + 1) * 256], hn_[:], sg[:, h * 256:(h + 1) * 256], ALU.mult, eng='pool')
                for dc in range(2):
                    k.mm(clp[:, dc, 0:257], kw[:, h, dc * 128:(dc + 1) * 128], vext[:, h, :])
                for dc in range(2):
                    Cs = C[:, h * 2 + dc, :]
                    k.ts(Cs, Cs, AB[:, 0, hc:hc + 1], ALU.mult)
                    k.stt(Cs, clp[:, dc, 0:257], AB[:, 1, hc:hc + 1], Cs, ALU.mult, ALU.add)
                    k.copy(Cb[:, h * 2 + dc, :], Cs, eng='act')
            k.dma(ya_s.ap()[c * 128:(c + 1) * 128, 1024:2048], yb[:], eng='pool')

        if not own:
            return tile
        for c in range(NC):
            tile(c)
        k.end_stage()

    def stage_mix0(self, ssd_args, ml_args):
        k = self
        k.begin_stage()
        ssd_tile = k.stage_ssd(*ssd_args, own=False)
        ml_tile = k.stage_mlstm(*ml_args, own=False)
        k.barrier()
        for i in range(self.NT):
            A = []; B = []
            k.rec = A; ssd_tile(i)
            k.rec = B; ml_tile(i)
            k.rec = None
            k.merge_replay(A, B, grain=getattr(k, 'x_grain', 1))
        k.end_stage()

    def load_w(self, w_ap, KC, N, name):
        W = self.sb([128, KC, N], BF16, name)
        for c in range(KC):
            self.dma(W[:, c, :], w_ap[c * 128:(c + 1) * 128, :], eng='pool')
        return W

    def stage_outproj(self, h_src, y_s, K, w_out, h_dst):
        k = self
        NT = self.NT
        KC = K // 128
        k.begin_stage()
        W = k.load_w(w_out, KC, 1024, "Wout")
        yts = k.ring(2, [128, K], BF16, "yt")
        hts = k.ring(2, [128, 1024], F32, "ht")
        yTs = k.ring(2, [128, KC, 128], BF16, "yT")
        hos = k.ring(2, [128, 1024], F32, "ho")
        tps = k.ring(2, [128, 8, 128], BF16, "tp", psum=True)
        pss = k.ring(4, [128, 512], F32, "pp", psum=True)
        for i in range(NT):
            yt = yts[i % 2]; ht = hts[i % 2]; yT = yTs[i % 2]; ho = hos[i % 2]
            k.dma(yt[:], y_s[i * 128:(i + 1) * 128, :])
            k.dma(ht[:], h_src[i * 128:(i + 1) * 128, :])
            for cg in range(KC // 8):
                tp = tps[cg % 2]
                for c in range(8):
                    k.tr(tp[:, c, :], yt[:, (cg * 8 + c) * 128:(cg * 8 + c + 1) * 128], self.ident[:])
                k.copy(yT[:, cg * 8:(cg + 1) * 8, :], tp[:], eng='act')
            for half in range(2):
                p = pss[(i * 2 + half) % 4]
                for c in range(KC):
                    k.mm(p[:], yT[:, c, :], W[:, c, half * 512:(half + 1) * 512], start=(c == 0), stop=(c == KC - 1))
                k.tt(ho[:, half * 512:(half + 1) * 512], p[:], ht[:, half * 512:(half + 1) * 512], ALU.add)
            k.dma(h_dst[i * 128:(i + 1) * 128, :], ho[:], eng='pool')
        k.end_stage()

    def merge_replay(self, A, B, grain=3):
        na, nb = len(A), len(B)
        steps = max(1, min(na, nb) // grain)
        for j in range(steps):
            self.replay(A[j * na // steps:(j + 1) * na // steps])
            self.replay(B[j * nb // steps:(j + 1) * nb // steps])

    def stage_xattn(self, h_src, h_dst, mem, g_x, g_m, wq, wkv, wo):
        k = self
        NT = self.NT
        ident = self.ident
        k.begin_stage()
        Wq = k.load_w(wq, 8, 1024, "Wq")
        Wo = k.load_w(wo, 8, 1024, "Wo")
        Wkv = k.load_w(wkv, 8, 2048, "Wkv")
        gx = k.sb([128, D], F32, "gx"); k.bcast_row(gx[:], g_x)
        gm = k.sb([128, D], F32, "gm"); k.bcast_row(gm[:], g_m)
        memT = k.sb([128, 8, 256], BF16, "memT")
        kT = k.sb([128, 8, 256], BF16, "kT")
        vx = k.sb([128, 2, 1024], BF16, "vx")

        def make_stream(sid):
            ht = k.sb([128, D], F32, "ht")
            ss = k.sb([128, 4], F32, "ss")
            hn = k.sb([128, D], BF16, "hn")
            hT = k.sb([128, 8, 128], BF16, "hT")
            qT = k.sb([128, 8, 128], BF16, "qT")
            Pm = k.sb([128, 4, 256], BF16, "Pm")
            s_ = k.sb([128, 16], F32, "s16")
            PTs = k.sb([128, 8, 128], BF16, "PTs")
            osb = k.sb([128, 1024], BF16, "osb")
            oT = k.sb([128, 8, 128], BF16, "oT")
            ho = k.sb([128, D], F32, "ho")
            bT = k.ps([128, 512], F32, "bT")
            bA = k.ps([128, 512], F32, "bA")
            bB = k.ps([128, 1024], F32, "bB")
            tp = bT[:].bitcast(BF16).rearrange("p (a b) -> p a b", a=8)
            bA4 = bA[:].rearrange("p (a b) -> p a b", a=4)
            bB4 = bB[:].rearrange("p (a b) -> p a b", a=4)
            st = dict(ht=ht, ss=ss, hn=hn, tp=tp, bA=bA, bB=bB)

            def tile(i):
                k.dma(ht[:], h_src[i * 128:(i + 1) * 128, :])
                k.rms_T(ht, gx, hn, hT[:], tp, ss, hn)
                for half in range(2):
                    for j4 in range(4):
                        j = half * 4 + j4
                        for c in range(8):
                            k.mm(bA4[:, j4, :], Wq[:, c, j * 128:(j + 1) * 128], hT[:, c, :], start=(c == 0), stop=(c == 7))
                    k.ts(qT[:, half * 4:(half + 1) * 4, :], bA4, 1.0 / 16, ALU.mult)
                for h in range(4):
                    for dc in range(2):
                        k.mm(bB4[:, h, :], qT[:, h * 2 + dc, :], kT[:, h * 2 + dc, :], start=(dc == 0), stop=(dc == 1))
                k.op('dve', 'tensor_reduce', out=s_[:, 0:4], in_=bB4, axis=AX.X, op=ALU.max)
                k.ts(s_[:, 4:8], s_[:, 0:4], -1.0, ALU.mult)
                for h in range(4):
                    k.act(Pm[:, h, :], bB4[:, h, :], AF.Exp, bias=s_[:, 4 + h:5 + h], accum_out=s_[:, 8 + h:9 + h])
                k.op('dve', 'reciprocal', out=s_[:, 12:16], in_=s_[:, 8:12])
                for h in range(4):
                    for mc in range(2):
                        k.tr(tp[:, h * 2 + mc, :], Pm[:, h, mc * 128:(mc + 1) * 128], ident[:])
                k.copy(PTs[:], tp, eng='act')
                for half in range(2):
                    for h2 in range(2):
                        h = half * 2 + h2
                        for mc in range(2):
                            k.mm(bA[:, h2 * 256:(h2 + 1) * 256], PTs[:, h * 2 + mc, :], vx[:, mc, h * 256:(h + 1) * 256],
                                 start=(mc == 0), stop=(mc == 1))
                    k.tt(osb[:, half * 512:(half + 1) * 512].rearrange("p (h e) -> p h e", h=2),
                         bA[:].rearrange("p (h e) -> p h e", h=2),
                         s_[:, 12 + half * 2:14 + half * 2].unsqueeze(2).to_broadcast([128, 2, 256]), ALU.mult)
                for c in range(8):
                    k.tr(tp[:, c, :], osb[:, c * 128:(c + 1) * 128], ident[:])
                k.copy(oT[:], tp, eng='act')
                for half in range(2):
                    reg = bB[:, half * 512:(half + 1) * 512]
                    for c in range(8):
                        k.mm(reg, oT[:, c, :], Wo[:, c, half * 512:(half + 1) * 512], start=(c == 0), stop=(c == 7))
                k.tt(ho[:], bB[:], ht[:], ALU.add)
                k.dma(h_dst[i * 128:(i + 1) * 128, :], ho[:], eng='pool')
            return tile, st

        t0, st0 = make_stream(0)
        t1, st1 = make_stream(1)
        for mc in range(2):
            k.dma(st0['ht'][:], mem[mc * 128:(mc + 1) * 128, :])
            k.rms_T(st0['ht'], gm, st0['hn'], memT[:, :, mc * 128:(mc + 1) * 128], st0['tp'], st0['ss'], st0['hn'])
        for j in range(8):
            reg = st0['bB'][:, (j % 4) * 256:(j % 4 + 1) * 256]
            for c in range(8):
                k.mm(reg, Wkv[:, c, j * 128:(j + 1) * 128], memT[:, c, :], start=(c == 0), stop=(c == 7))
            k.copy(kT[:, j, :], reg, eng='act')
        for mc in range(2):
            for half in range(2):
                reg = st1['bB'][:, half * 512:(half + 1) * 512]
                for c in range(8):
                    k.mm(reg, memT[:, c, mc * 128:(mc + 1) * 128], Wkv[:, c, 1024 + half * 512:1024 + (half + 1) * 512],
                         start=(c == 0), stop=(c == 7))
                k.copy(vx[:, mc, half * 512:(half + 1) * 512], reg)
        if NT % 2 == 0 and NT >= 2:
            H2 = NT // 2
            for i in range(H2):
                A = []; B = []
                k.rec = A; t0(i)
                k.rec = B; t1(i + H2)
                k.rec = None
                k.merge_replay(A, B, grain=getattr(k, 'x_grain', 1))
        else:
            for i in range(NT):
                t0(i)
        k.end_stage()

    def precast_ops(self, pairs):
        k = self
        st = k.ring(3, [128, 2, 1024], F32, "pc32")
        sb = k.ring(3, [128, 2, 1024], BF16, "pc16")
        ops = []
        k.rec = ops
        n = 0
        for (src, dst) in pairs:
            R_ = src.shape[0]
            for r0 in range(0, R_, 256):
                a = st[n % 3]; b = sb[n % 3]
                k.dma(a[:], src[r0:r0 + 256, :].rearrange("(p j) n -> p j n", j=2))
                eng = ('dve', 'act', 'dve', 'pool')[n % 4]
                k.copy(b[:], a[:], eng=eng)
                k.dma(dst[r0:r0 + 256, :].rearrange("(p j) n -> p j n", j=2), b[:], eng='pool')
                n += 1
        k.rec = None
        return ops

    def stage_precast(self, pairs):
        k = self
        k.begin_stage()
        k.replay(k.precast_ops(pairs))
        k.end_stage()

    def stage_peer(self, h_src, h_dst, g_f, wq, skT, uR, vtab, final_gamma=None):
        k = self
        T = self.T
        TP = 256 if T >= 256 else 128
        NS = TP // 128
        NTP = T // TP
        idf, ident = self.idf, self.ident
        self._uid += 1
        gtd = [[k.dram("gtd%d_%d_%d" % (j, ig, self._uid), [16, 128, TP], BF16).ap() for ig in range(8)]
               for j in range(2)]
        k.begin_stage()
        Wq = k.load_w(wq, 8, 2048, "Wpq")
        sk = k.sb([128, 16, 128], BF16, "sk")
        k.dma(sk[:], skT, eng='pool')
        gf = k.sb([128, D], F32, "gf"); k.bcast_row(gf[:], g_f)
        if final_gamma is not None:
            gfin = k.sb([128, D], F32, "gfin"); k.bcast_row(gfin[:], final_gamma)
        ioti = k.sb([128, 128], I32, "ioti")
        k.op('pool', 'iota', out=ioti[:], pattern=[[1, 128]], base=0, channel_multiplier=0)
        iotf = k.sb([128, 128], F32, "iotf")
        k.copy(iotf[:], ioti[:])
        iotb = k.sb([128, 128], BF16, "iotb")
        k.copy(iotb[:], ioti[:])
        GT = k.sb([128, 128, TP], BF16, "GT", gran=16 * TP)
        xTs = k.ring(2, [128, 8, TP], BF16, "xT")
        qT = k.sb([128, 16, TP], BF16, "qT", gran=TP)
        hts = k.ring(2, [128, D], F32, "ht")
        hn = k.sb([128, D], BF16, "hn")
        ss = k.sb([128, 4], F32, "ss")
        ss2 = k.sb([128, 4], F32, "ss2")
        V = k.sb([128, 16, 16], F32, "V", gran=8)
        IX = k.sb([128, 16, 16], U32, "IX", gran=8)
        IXf = k.sb([128, 16, 16], F32, "IXf")
        wk = k.sb([128, 2, 128], F32, "wk", gran=128)
        cand = k.sb([128, 8, 16, 16], F32, "cand", gran=256)
        eq = cand
        wk2 = k.sb([128, 2, 256], F32, "wk2", gran=256)
        TSs = k.ring(2, [128, 8, 16], F32, "TS", gran=8)
        PX = k.sb([128, 8, 16], U32, "PX", gran=8)
        PAB = k.sb([128, 2, 8, 16], U32, "PAB")
        PABf = k.sb([128, 2, 8, 16], F32, "PABf")
        I12Ws = k.ring(2, [128, 3, 128], F32, "I12W", gran=128)
        sgs = k.ring(2, [128, 32], F32, "sg", gran=1)
        Eg = k.sb([128, 8, 16], F32, "Eg", gran=16)
        IT = k.sb([128, 3, TP], F32, "IT")
        Pms = k.sb([128, 16, 128], BF16, "Pm", gran=128)
        Qms = k.sb([128, 16, 128], BF16, "Qm", gran=128)
        NR = 3
        urs = k.ring(NR, [128, 2, 1024], BF16, "ur")
        vrs = k.ring(NR, [128, 2, 1024], BF16, "vr")
        gtr = k.ring(NR, [128, 2, TP], BF16, "gtr")
        gls = k.ring(3, [128, TP], F32, "gl")
        GAs = k.ring(3, [128, TP], BF16, "GA")
        hos = k.ring(1, [128, D], F32, "ho")
        big4 = k.ps([128, 4, 512], F32, "big4")
        r2 = k.ps([128, 2, 512], F32, "r2")
        ab = k.ring(2, [128, 512], F32, "ab", psum=True)
        r2f = r2[:].rearrange("p a b -> p (a b)")
        Vv = V[:].rearrange("p (h q) k -> p h q k", q=2)
        IXv = IXf[:].rearrange("p (h q) k -> p h q k", q=2)
        B4 = [128, 8, 16, 16]
        cnt = {'r2': 0, 'ab': 0}

        def r2s(width):
            j = cnt['r2'] % 4; cnt['r2'] += 1
            return r2f[:, j * 256:j * 256 + width]

        def abn():
            j = cnt['ab'] % 2; cnt['ab'] += 1
            return ab[j]

        def phase_ab(n):
            t0 = n * TP
            xT = xTs[n % 2]
            for st in range(NS):
                ht = hts[st % 2]
                k.dma(ht[:], h_src[t0 + st * 128:t0 + (st + 1) * 128, :])
                tpb = abn()[:].bitcast(BF16).rearrange("p (a b) -> p a b", a=8)
                k.act(hn[:], ht[:], AF.Square, accum_out=ss[:, 0:1])
                k.ts(ss[:, 1:2], ss[:, 0:1], 1.0 / D, ALU.mult, EPS, ALU.add)
                k.tt(ss[:, 3:4], ss[:, 1:2], self.mhalf[:, 0:1], ALU.pow, eng='pool')
                k.stt(hn[:], ht[:], ss[:, 3:4], gf[:], ALU.mult, ALU.mult)
                for c in range(8):
                    k.tr(tpb[:, c, :], hn[:, c * 128:(c + 1) * 128], ident[:])
                k.copy(xT[:, :, st * 128:(st + 1) * 128], tpb, eng='act')
            for hp in range(16):
                p = abn()[:, 0:TP]
                for c in range(8):
                    k.mm(p, Wq[:, c, hp * 128:(hp + 1) * 128], xT[:, c, :], start=(c == 0), stop=(c == 7))
                k.copy(qT[:, hp, :], p, eng=('act' if hp % 2 else 'dve'))
            def it_transposes(st):
                I12 = I12Ws[st % 2]
                p = abn()
                for j in range(3):
                    k.tr(p[:, j * 128:(j + 1) * 128], I12[:, j, :], idf[:])
                k.copy(IT[:, :, st * 128:(st + 1) * 128], p[:, 0:384].rearrange("p (a b) -> p a b", a=3), eng='act')

            def gbuild(ta, tb_):
                NG = (tb_ - ta) // 4
                p4s = {}
                for it in range(NG + 3):
                    g_ts = it; g_mm = it - 2; g_cp = it - 3
                    if g_ts < NG:
                        for tt_ in range(4):
                            t = ta + g_ts * 4 + tt_
                            sl_ = t % 16
                            k.ts(Pms[:, sl_, :], iotb[:], IT[:, 0, t:t + 1], ALU.is_equal, eng=('pool' if t % self.x_pool_every == 0 else 'dve'))
                            k.ts(Qms[:, sl_, :], iotb[:], IT[:, 1, t:t + 1], ALU.is_equal, IT[:, 2, t:t + 1], ALU.mult)
                    if 0 <= g_mm < NG:
                        p4 = abn()[:].rearrange("p (a b) -> p a b", a=4)
                        p4s[g_mm] = p4
                        for tt_ in range(4):
                            sl_ = (ta + g_mm * 4 + tt_) % 16
                            k.mm(p4[:, tt_, :], Qms[:, sl_, :], Pms[:, sl_, :])
                    if 0 <= g_cp < NG:
                        tq = ta + g_cp * 4
                        k.copy(GT[:, :, tq:tq + 4], p4s.pop(g_cp).rearrange("p t i -> p i t"),
                               eng=('dve' if g_cp % 4 == 0 else 'act'))

            def gate(st):
                I12W = I12Ws[st % 2]; TS = TSs[st % 2]; sg = sgs[st % 2]
                k.ts(sg[:, 0:8], TS[:, :, 0], -1.0, ALU.mult)
                for h in range(8):
                    k.act(Eg[:, h, :], TS[:, h, :], AF.Exp, bias=sg[:, h:h + 1], accum_out=sg[:, 8 + h:9 + h])
                k.op('dve', 'reciprocal', out=sg[:, 16:24], in_=sg[:, 8:16])
                k.tt(I12W[:, 2, :].rearrange("p (h k) -> p h k", h=8), Eg[:], sg[:, 16:24].unsqueeze(2).to_broadcast([128, 8, 16]), ALU.mult)

            for st in range(NS):
                I12W = I12Ws[st % 2]; TS = TSs[st % 2]
                for g0 in range(0, 16, 4):
                    hps = range(g0, g0 + 4)
                    S4 = abn()[:].rearrange("p (a b) -> p a b", a=4)
                    for hp in hps:
                        k.mm(S4[:, hp % 4, :], qT[:, hp, st * 128:(st + 1) * 128], sk[:, hp, :])
                    for hps2 in (range(g0, g0 + 2), range(g0 + 2, g0 + 4)):
                        for hp in hps2:
                            k.op('dve', 'max', out=V[:, hp, 0:8], in_=S4[:, hp % 4, :])
                        for hp in hps2:
                            k.op('dve', 'max_index', out=IX[:, hp, 0:8], in_max=V[:, hp, 0:8], in_values=S4[:, hp % 4, :])
                        for hp in hps2:
                            k.op('dve', 'match_replace', out=wk[:, hp % 2, :], in_to_replace=V[:, hp, 0:8], in_values=S4[:, hp % 4, :], imm_value=-1e30)
                        for hp in hps2:
                            k.op('dve', 'max', out=V[:, hp, 8:16], in_=wk[:, hp % 2, :])
                        for hp in hps2:
                            k.op('dve', 'max_index', out=IX[:, hp, 8:16], in_max=V[:, hp, 8:16], in_values=wk[:, hp % 2, :])
                k.copy(IXf[:], IX[:])
                k.tt(cand[:], Vv[:, :, 0, :].unsqueeze(3).to_broadcast(B4), Vv[:, :, 1, :].unsqueeze(2).to_broadcast(B4), ALU.add)
                cvs = [cand[:, h, :, :].rearrange("p a b -> p (a b)") for h in range(8)]
                for g0 in range(0, 8, 2):
                    hsr = range(g0, g0 + 2)
                    for h in hsr:
                        k.op('dve', 'max', out=TS[:, h, 0:8], in_=cvs[h])
                    for h in hsr:
                        k.op('dve', 'max_index', out=PX[:, h, 0:8], in_max=TS[:, h, 0:8], in_values=cvs[h])
                    for h in hsr:
                        k.op('dve', 'match_replace', out=wk2[:, h % 2, :], in_to_replace=TS[:, h, 0:8], in_values=cvs[h], imm_value=-1e30)
                    for h in hsr:
                        k.op('dve', 'max', out=TS[:, h, 8:16], in_=wk2[:, h % 2, :])
                    for h in hsr:
                        k.op('dve', 'max_index', out=PX[:, h, 8:16], in_max=TS[:, h, 8:16], in_values=wk2[:, h % 2, :])
                k.op('dve', 'tensor_single_scalar', out=PAB[:, 0, :, :], in_=PX[:], scalar=4, op=ALU.logical_shift_right)
                k.op('dve', 'tensor_single_scalar', out=PAB[:, 1, :, :], in_=PX[:], scalar=15, op=ALU.bitwise_and)
                k.copy(PABf[:], PAB[:])
                for q in range(2):
                    k.tt(eq[:], iotf[:, 0:16].unsqueeze(1).unsqueeze(1).to_broadcast(B4),
                         PABf[:, q, :, :].unsqueeze(3).to_broadcast(B4), ALU.is_equal)
                    k.tt(eq[:], eq[:], IXv[:, :, q, :].unsqueeze(2).to_broadcast(B4), ALU.mult)
                    k.op('dve', 'tensor_reduce', out=I12W[:, q, :].rearrange("p (h k) -> p h k", h=8), in_=eq[:], axis=AX.X, op=ALU.add)
            if k.rec is not None:
                k.rec.append(('marker', 34, None, 0))
            for st in range(NS):
                gate(st)
                it_transposes(st)
                gbuild(st * 128, (st + 1) * 128)
            for ig in range(2, 8):
                k.dma(gtd[n % 2][ig].rearrange("i j t -> j i t"), GT[:, ig * 16:(ig + 1) * 16, :], eng='act')

        def dense(n, side):
            xT = xTs[n % 2]
            g_ = gtd[n % 2]

            def load(i):
                sl_ = (i // 2) % NR
                k.dma(urs[sl_][:], uR[i:i + 2].rearrange("i p n -> p i n"))
                k.dma(vrs[sl_][:], vtab[i * 128:(i + 2) * 128, :].rearrange("(i p) n -> p i n", p=128))
                if i >= 32:
                    k.dma(gtr[sl_][:], g_[i // 16][i % 16:i % 16 + 2].rearrange("i j t -> j i t"))

            def uphase(i):
                sl_ = (i // 2) % NR
                p = r2s(TP)
                for c in range(8):
                    k.mm(p, urs[sl_][:, i % 2, c * 128:(c + 1) * 128], xT[:, c, :], start=(c == 0), stop=(c == 7))
                gl = gls[i % 3]; GA = GAs[i % 3]
                k.act(gl[:], p, AF.Gelu)
                k.tt(GA[:], gl[:], (GT[:, i, :] if i < 32 else gtr[sl_][:, i % 2, :]), ALU.mult, eng='pool')

            def vphase(i):
                vr = vrs[(i // 2) % NR]; GA = GAs[i % 3]
                for st in range(NS):
                    for dh in range(2):
                        k.mm(big4[:, st * 2 + dh, :], GA[:, st * 128:(st + 1) * 128], vr[:, i % 2, dh * 512:(dh + 1) * 512],
                             start=(i == 0), stop=(i == 127))

            tot_cost = sum(o[3] for o in side)
            per_cost = tot_cost / float(getattr(k, 'x_div', 120)) + 1
            for i in range(0, 2 * (NR - 1), 2):
                load(i)
            uphase(0)
            for i in range(128):
                if i % 2 == 0 and i + 2 * (NR - 1) < 128:
                    load(i + 2 * (NR - 1))
                if i + 1 < 128:
                    uphase(i + 1)
                vphase(i)
                side = k.replay_cost(side, per_cost, chunk=i)
            k.replay(side)
            t0 = n * TP
            for st in range(NS):
                ho = hos[0]; ht = hts[st % 2]
                k.dma(ht[:], h_src[t0 + st * 128:t0 + (st + 1) * 128, :])
                k.tt(ho[:], big4[:, st * 2:st * 2 + 2, :].rearrange("p a b -> p (a b)"), ht[:], ALU.add)
                if final_gamma is not None:
                    k.act(hn[:], ho[:], AF.Square, accum_out=ss2[:, 0:1])
                    k.ts(ss2[:, 1:2], ss2[:, 0:1], 1.0 / D, ALU.mult, EPS, ALU.add)
                    k.tt(ss2[:, 3:4], ss2[:, 1:2], self.mhalf[:, 0:1], ALU.pow, eng='pool')
                    k.stt(ho[:], ho[:], ss2[:, 3:4], gfin[:], ALU.mult, ALU.mult)
                k.dma(h_dst[t0 + st * 128:t0 + (st + 1) * 128, :], ho[:], eng='pool')

        phase_ab(0)
        for n in range(NTP):
            side = []
            if n + 1 < NTP:
                k.rec = side
                phase_ab(n + 1)
                k.rec = None
            dense(n, side)
        k.end_stage()

    def stage_hgrn(self, h_src, h_dst, gamma, w_in, w_out, lb_logits, hnorm):
        k = self
        NT = self.NT
        ident, triu, ones = self.ident, self.triu, self.ones
        k.begin_stage()
        Win = k.load_w(w_in, 8, 4096, "Win")
        Wout = k.load_w(w_out, 8, 1024, "Wout")
        gb = k.sb([128, D], F32, "gb"); k.bcast_row(gb[:], gamma)
        hnb = k.sb([128, D], F32, "hnb"); k.bcast_row(hnb[:], hnorm)
        lb = k.sb([128, D], F32, "lb"); k.bcast_row(lb[:], lb_logits[0])
        oml = k.sb([128, D], F32, "oml"); k.bcast_row(oml[:], lb_logits[1])
        k.tt(lb[:], lb[:], oml[:], ALU.subtract)
        k.act(lb[:], lb[:], AF.Sigmoid)
        k.ts(oml[:], lb[:], -1.0, ALU.mult, 1.0, ALU.add)
        A1 = k.sb([128, 128], F32, "A1")
        k.op('pool', 'affine_select', out=A1[:], in_=ones[:], pattern=[[0, 128]], compare_op=ALU.is_ge, fill=0.0,
             base=64, channel_multiplier=-1)
        k.tt(A1[:], triu[:], A1[:], ALU.subtract)
        A3 = k.sb([128, 128], F32, "A3")
        k.tt(A3[:], ones[:], triu[:], ALU.subtract)
        S = k.sb([128, 8, 128], F32, "S"); k.memset(S[:], 0.0)
        Sb = k.sb([128, 8, 128], BF16, "Sb"); k.memset(Sb[:], 0.0)
        hts = k.ring(2, [128, D], F32, "ht")
        ss = k.sb([128, 4], F32, "ss")
        hn = k.sb([128, D], BF16, "hn")
        hTs = k.ring(2, [128, 8, 128], BF16, "hT")
        qs = k.ring(2, [128, D], F32, "q")
        fs = k.ring(2, [128, D], F32, "f")
        sgts = k.ring(2, [128, D], F32, "sgt")
        vs = k.ring(2, [128, D], BF16, "v")
        logf = k.sb([128, D], F32, "logf")
        kk = k.sb([128, D], F32, "kk")
        ex = k.sb([128, D], F32, "ex")
        qa = k.sb([128, D], BF16, "qa"); ka = k.sb([128, D], BF16, "ka")
        qg = k.sb([128, D], BF16, "qg"); kl = k.sb([128, D], BF16, "kl")
        qaT = k.sb([128, 8, 128], BF16, "qaT"); kaT = k.sb([128, 8, 128], BF16, "kaT"); qgT = k.sb([128, 8, 128], BF16, "qgT")
        cd = k.sb([128, 8], F32, "cd")
        attm4s = k.ring(2, [128, 4, 128], BF16, "attm4")
        o1 = k.sb([128, D], F32, "o1")
        s8 = k.sb([128, 32], F32, "s8")
        on = k.sb([128, D], BF16, "on")
        oT = k.sb([128, 8, 128], BF16, "oT")
        hos = k.ring(2, [128, D], F32, "ho")
        prA = k.ps([128, 512], F32, "prA")
        tpAf = k.ps([128, 512], F32, "tpA")
        tpA = tpAf[:].bitcast(BF16).rearrange("p (a b) -> p a b", a=8)
        pG = k.ps([128, 2, 512], F32, "pG")
        tp = k.ps([128, 8, 128], BF16, "tp")
        attp = k.ps([128, 512], F32, "attp")
        o_ps = k.ps([128, 1024], F32, "o_ps")
        sl = pG[:].rearrange("p a (b c) -> p (a b) c", c=128)

        def v8(ap):
            return ap.rearrange("p (h d) -> p h d", h=8)

        def prefix(i):
            ht = hts[i % 2]; hT = hTs[i % 2]
            q = qs[i % 2]; f = fs[i % 2]; v = vs[i % 2]; sgt = sgts[i % 2]
            k.dma(ht[:], h_src[i * 128:(i + 1) * 128, :])
            k.rms_T(ht, gb, hn, hT[:], tpA, ss, hn)

            def proj(col0, fn):
                for half in range(2):
                    for c in range(8):
                        k.mm(prA[:], hT[:, c, :], Win[:, c, col0 + half * 512:col0 + (half + 1) * 512], start=(c == 0), stop=(c == 7))
                    fn(prA, slice(half * 512, (half + 1) * 512))
            proj(0, lambda p, sl_: k.act(q[:, sl_], p[:], AF.Silu))
            proj(3072, lambda p, sl_: k.act(sgt[:, sl_], p[:], AF.Silu))
            proj(1024, lambda p, sl_: k.act(f[:, sl_], p[:], AF.Sigmoid))
            proj(2048, lambda p, sl_: k.copy(v[:, sl_], p[:]))

        def rest(i):
            ht = hts[i % 2]; ho = hos[i % 2]
            q = qs[i % 2]; f = fs[i % 2]; v = vs[i % 2]; sgt = sgts[i % 2]
            k.tt(f[:], f[:], oml[:], ALU.mult)
            k.tt(f[:], f[:], lb[:], ALU.add)
            k.act(logf[:], f[:], AF.Ln)
            k.ts(kk[:], f[:], -1.0, ALU.mult, 1.0, ALU.add)
            pGf = pG[:].rearrange("p a b -> p (a b)")
            for half in range(2):
                k.mm(pG[:, half, :], A1[:], logf[:, half * 512:(half + 1) * 512])
            k.act(ex[:], pGf, AF.Exp)
            k.tt(qa[:], q[:], ex[:], ALU.mult)
            k.act(ex[:], pGf, AF.Exp, scale=-1.0)
            k.tt(ka[:], kk[:], ex[:], ALU.mult)
            for half in range(2):
                k.mm(pG[:, half, :], triu[:], logf[:, half * 512:(half + 1) * 512])
            k.act(ex[:], pGf, AF.Exp)
            k.tt(qg[:], q[:], ex[:], ALU.mult)
            for half in range(2):
                k.mm(pG[:, half, :], A3[:], logf[:, half * 512:(half + 1) * 512])
            k.act(ex[:], pGf, AF.Exp)
            k.tt(kl[:], kk[:], ex[:], ALU.mult)
            for h in range(8):
                k.mm(attp[:, 256 + h:257 + h], logf[:, h * 128:(h + 1) * 128], ones[:, 0:1])
            k.act(cd[:], attp[:, 256:264], AF.Exp)
            for (src, dst) in ((qa, qaT), (ka, kaT), (qg, qgT)):
                for c in range(8):
                    k.tr(tp[:, c, :], src[:, c * 128:(c + 1) * 128], ident[:])
                k.copy(dst[:], tp[:], eng='act')
            attp4 = attp[:].rearrange("p (a b) -> p a b", a=4)
            for hq in range(2):
                attm4 = attm4s[hq]
                for h4 in range(4):
                    h = hq * 4 + h4
                    k.mm(attp4[:, h4, :], kaT[:, h, :], qaT[:, h, :])
                k.tt(attm4[:], attp4, triu[:].unsqueeze(1).to_broadcast([128, 4, 128]), ALU.mult)
                for h4 in range(4):
                    h = hq * 4 + h4
                    hs = slice(h * 128, (h + 1) * 128)
                    k.mm(o_ps[:, hs], attm4[:, h4, :], v[:, hs], start=True, stop=False)
                    k.mm(o_ps[:, hs], qgT[:, h, :], Sb[:, h, :], start=False, stop=True)
                    k.mm(sl[:, h, :], kl[:, hs], v[:, hs])
            for h in range(8):
                k.stt(S[:, h, :], S[:, h, :], cd[:, h:h + 1], sl[:, h, :], ALU.mult, ALU.add)
            k.copy(Sb[:], S[:], eng='act')
            k.act(ex[:], o_ps[:], AF.Square)
            k.op('dve', 'tensor_reduce', out=s8[:, 0:8], in_=v8(ex[:]), axis=AX.X, op=ALU.add)
            k.ts(s8[:, 8:16], s8[:, 0:8], 1.0 / 128, ALU.mult, EPS, ALU.add)
            k.tt(s8[:, 24:32], s8[:, 8:16], self.mhalf[:, 0:8], ALU.pow, eng='pool')
            k.tt(v8(o1[:]), v8(o_ps[:]), s8[:, 24:32].unsqueeze(2).to_broadcast([128, 8, 128]), ALU.mult)
            k.tt(o1[:], o1[:], hnb[:], ALU.mult, eng='pool')
            k.tt(on[:], o1[:], sgt[:], ALU.mult)
            for c in range(8):
                k.tr(tp[:, c, :], on[:, c * 128:(c + 1) * 128], ident[:])
            k.copy(oT[:], tp[:], eng='act')
            for half in range(2):
                for c in range(8):
                    k.mm(pG[:, half, :], oT[:, c, :], Wout[:, c, half * 512:(half + 1) * 512], start=(c == 0), stop=(c == 7))
                k.tt(ho[:, half * 512:(half + 1) * 512], pG[:, half, :], ht[:, half * 512:(half + 1) * 512], ALU.add)
            k.dma(h_dst[i * 128:(i + 1) * 128, :], ho[:], eng='pool')

        prefix(0)
        for i in range(NT):
            A = []; B = []
            k.rec = A; rest(i)
            if i + 1 < NT:
                k.rec = B; prefix(i + 1)
            k.rec = None
            if B:
                k.merge_replay(A, B, grain=getattr(k, 'x_grain', 1))
            else:
                k.replay(A)
        k.end_stage()


def build_full(T=4096, dbg=()):
    k = MK(T, dbg=dbg)
    I = {}
    def inp(name, shape, dt=F32):
        I[name] = k.inp(name, shape, dt).ap()
        return I[name]
    inp("x", [T, 1024]); inp("mem", [256, 1024])
    for n in ("norm_mix", "norm_xattn", "norm_mem", "norm_ffn", "hgrn_lb_logits"):
        inp(n, [2, 1024])
    inp("norm_final", [1024])
    inp("ev_w_in", [1024, E_IN]); inp("ev_w_out", [2048, 1024]); inp("convwb", [2560, 5])
    for n in ("ssd_dt_bias", "ssd_a_log", "ssd_d_skip"):
        inp(n, [16])
    inp("ssd_norm", [1024])
    for n in ("ml_wq", "ml_wk", "ml_wv"):
        inp(n, [4, 256, 256])
    inp("ml_i_bias", [4]); inp("ml_f_bias", [4]); inp("ml_norm", [1024])
    inp("od_w_in", [1024, 4096]); inp("od_w_out", [1024, 1024]); inp("hgrn_norm", [1024])
    inp("xa_wq", [2, 1024, 1024]); inp("xa_wkv", [2, 1024, 2048]); inp("xa_wo", [2, 1024, 1024])
    inp("peer_wq", [2, 1024, 2048]); inp("peer_skT", [2, 128, 16, 128])
    inp("peer_uR", [2, 16384, 1024]); inp("peer_v", [2, 16384, 1024])
    out = k.dram("out", [T, 1024], F32, kind="ExternalOutput").ap()
    k.make_consts()
    ub = [k.scratch("ub%d_s" % l, [16384, 1024], BF16).ap() for l in range(2)]
    vb = [k.scratch("vb%d_s" % l, [16384, 1024], BF16).ap() for l in range(2)]
    hs = [k.scratch("h%d_s" % j, [T, 1024]).ap() for j in range(1, 6)]
    pairs = [(I["peer_uR"][l], ub[l]) for l in range(2)] + [(I["peer_v"][l], vb[l]) for l in range(2)]
    k.stage_inproj_even(I["x"], I["ev_w_in"], I["norm_mix"][0], side_fn=lambda: k.precast_ops(pairs))
    k.stage_mix0((I["convwb"], I["ssd_dt_bias"], I["ssd_a_log"], I["ssd_d_skip"], I["ssd_norm"]),
                 (I["convwb"], I["ml_wq"], I["ml_wk"], I["ml_wv"], I["ml_i_bias"], I["ml_f_bias"], I["ml_norm"]))
    k.stage_outproj(I["x"], k.scr["ya_s"].ap(), 2048, I["ev_w_out"], hs[0])
    k.stage_xattn(hs[0], hs[1], I["mem"], I["norm_xattn"][0], I["norm_mem"][0], I["xa_wq"][0], I["xa_wkv"][0], I["xa_wo"][0])
    k.stage_peer(hs[1], hs[2], I["norm_ffn"][0], I["peer_wq"][0], I["peer_skT"][0],
                 ub[0].rearrange("(i p) n -> i p n", p=128), vb[0])
    k.stage_hgrn(hs[2], hs[3], I["norm_mix"][1], I["od_w_in"], I["od_w_out"], I["hgrn_lb_logits"], I["hgrn_norm"])
    k.stage_xattn(hs[3], hs[4], I["mem"], I["norm_xattn"][1], I["norm_mem"][1], I["xa_wq"][1], I["xa_wkv"][1], I["xa_wo"][1])
    k.stage_peer(hs[4], out, I["norm_ffn"][1], I["peer_wq"][1], I["peer_skT"][1],
                 ub[1].rearrange("(i p) n -> i p n", p=128), vb[1], final_gamma=I["norm_final"])
    return k


def prep_shared(inputs):
    f = lambda a: np.ascontiguousarray(np.asarray(a, dtype=np.float32))
    P = inputs
    convwb = np.concatenate([
        np.concatenate([np.asarray(P['ssd_conv_w'])[0].T, np.asarray(P['ssd_conv_b'])[0][:, None]], 1),
        np.concatenate([np.asarray(P['ml_conv_w'])[0].T, np.asarray(P['ml_conv_b'])[0][:, None]], 1)], 0)
    sh = {
        "norm_mix": f(P['norm_mix']), "norm_xattn": f(P['norm_xattn']), "norm_mem": f(P['norm_mem']),
        "norm_ffn": f(P['norm_ffn']), "hgrn_lb_logits": f(P['hgrn_lb_logits']), "norm_final": f(P['norm_final']),
        "ev_w_in": f(np.asarray(P['ev_w_in'])[0]), "ev_w_out": f(np.asarray(P['ev_w_out'])[0]), "convwb": f(convwb),
        "ssd_dt_bias": f(np.asarray(P['ssd_dt_bias'])[0]), "ssd_a_log": f(np.asarray(P['ssd_a_log'])[0]),
        "ssd_d_skip": f(np.asarray(P['ssd_d_skip'])[0]), "ssd_norm": f(np.asarray(P['ssd_norm'])[0]),
        "ml_wq": f(np.asarray(P['ml_wq'])[0]), "ml_wk": f(np.asarray(P['ml_wk'])[0]), "ml_wv": f(np.asarray(P['ml_wv'])[0]),
        "ml_i_bias": f(np.asarray(P['ml_i_bias'])[0]), "ml_f_bias": f(np.asarray(P['ml_f_bias'])[0]),
        "ml_norm": f(np.asarray(P['ml_norm'])[0]),
        "od_w_in": f(np.asarray(P['od_w_in'])[0]), "od_w_out": f(np.asarray(P['od_w_out'])[0]),
        "hgrn_norm": f(np.asarray(P['hgrn_norm'])[0]),
        "xa_wq": f(P['xa_wq']), "xa_wkv": f(P['xa_wkv']), "xa_wo": f(P['xa_wo']),
        "peer_wq": f(P['peer_wq']),
        "peer_skT": f(np.asarray(P['peer_subkeys']).transpose(0, 4, 1, 2, 3).reshape(2, 128, 16, 128)),
        "peer_uR": f(np.asarray(P['peer_u']).reshape(2, 128, 128, 8, 128).transpose(0, 1, 4, 3, 2).reshape(2, 16384, 1024)),
        "peer_v": f(P['peer_v']),
    }
    return sh


def kernel(**inputs):
    x = np.asarray(inputs['x'], dtype=np.float32)
    mem = np.asarray(inputs['mem'], dtype=np.float32)
    B, T, _ = x.shape
    sh = prep_shared(inputs)
    k = build_full(T)
    in_maps = []
    for b in range(B):
        m = dict(sh)
        m["x"] = np.ascontiguousarray(x[b])
        m["mem"] = np.ascontiguousarray(mem[b])
        in_maps.append(m)
    res = run_bass_kernel_spmd(k.nc, in_maps, core_ids=list(range(B)))
    return np.stack([np.asarray(res.results[b]["out"], dtype=np.float32) for b in range(B)], 0)
```
